# Optimizing a Trainium2 kernel written in Bass

```python
import math
import jax, jax.numpy as jnp
from jax import lax
import numpy as np

D_MODEL = 1024
BATCH = 4
SEQ = 4096
DEPTH = 1

MEM_LEN = 256
POOL_WINDOWS = (2, 4, 8, 16)
POOL_GROUPS = 4
POOL_GROUP_DIM = D_MODEL // 8
POOL_DIM = POOL_GROUPS * POOL_GROUP_DIM
MLA_HEADS = 8
QK_NOPE_DIM = 128
QK_ROPE_DIM = 64
V_HEAD_DIM = 128
Q_LORA_RANK = 384
KV_LORA_RANK = 256
MLA_V_DIM = MLA_HEADS * V_HEAD_DIM
ROPE_THETA = 10000.0
Q_BLOCK = 128
XATTN_HEADS = 4
XATTN_HEAD_DIM = 128
XATTN_DIM = XATTN_HEADS * XATTN_HEAD_DIM
N_BRANCHES = 3
IN_SPLIT_SIZES = (POOL_DIM, Q_LORA_RANK, KV_LORA_RANK, QK_ROPE_DIM, XATTN_DIM, N_BRANCHES * D_MODEL)
D_IN = sum(IN_SPLIT_SIZES)
N_GROUPS = 4
EXPERTS_PER_GROUP = 8
N_EXPERTS = N_GROUPS * EXPERTS_PER_GROUP
TOP_K = 2
D_EXPERT = D_MODEL // 4
MOE_BLOCK = 128
RMS_EPS = 1e-6
NEG_INF = -1e30

kernel_name = "hybrid_pool_mla_memxattn_hmoe"


def rms_norm(x, g):
    xf = x.astype(jnp.float32)
    xf = xf * lax.rsqrt(jnp.mean(xf * xf, axis=-1, keepdims=True) + RMS_EPS)
    return xf.astype(x.dtype) * g


def rope_tables(positions):
    inv_freq = 1.0 / (ROPE_THETA ** (jnp.arange(0, QK_ROPE_DIM, 2, dtype=jnp.float32) / QK_ROPE_DIM))
    ang = positions.astype(jnp.float32)[..., None] * inv_freq
    return jnp.cos(ang), jnp.sin(ang)


def apply_rope(x, cos, sin):
    xf = x.astype(jnp.float32)
    half = xf.shape[-1] // 2
    x1, x2 = xf[..., :half], xf[..., half:]
    return jnp.concatenate([x1 * cos - x2 * sin, x2 * cos + x1 * sin], axis=-1).astype(x.dtype)


def pool_mixer(u, pool_w, pool_scale):
    B, S, _ = u.shape
    uf = u.astype(jnp.float32)
    cs = jnp.pad(jnp.cumsum(uf, axis=1), ((0, 0), (1, 0), (0, 0)))
    cnt_base = jnp.arange(S) + 1
    outs = []
    for g, w in enumerate(POOL_WINDOWS):
        sl = slice(g * POOL_GROUP_DIM, (g + 1) * POOL_GROUP_DIM)
        cg = cs[..., sl]
        lag = jnp.pad(cg[:, :S + 1 - w], ((0, 0), (w - 1, 0), (0, 0)))
        cnt = jnp.minimum(cnt_base, w).astype(jnp.float32)[None, :, None]
        outs.append((cg[:, 1:] - lag) / cnt - uf[..., sl])
    p = jnp.stack(outs, axis=2).astype(u.dtype)
    y = jnp.einsum('bsgc,gcd->bsgd', p, pool_w).reshape(B, S, POOL_DIM)
    return y * pool_scale


def causal_block_attention(q_nope, q_rope, k_nope, k_rope, v):
    B, S, H, _ = q_nope.shape
    nb = S // Q_BLOCK
    scale = (QK_NOPE_DIM + QK_ROPE_DIM) ** -0.5
    k_idx = jnp.arange(S)

    def one_block(args):
        qn, qr, i = args
        s = (jnp.einsum('bqhd,bkhd->bhqk', qn, k_nope)
             + jnp.einsum('bqhr,bkr->bhqk', qr, k_rope)).astype(jnp.float32) * scale
        q_idx = i * Q_BLOCK + jnp.arange(Q_BLOCK)
        s = jnp.where(k_idx[None, :] <= q_idx[:, None], s, NEG_INF)
        p = jax.nn.softmax(s, axis=-1).astype(v.dtype)
        return jnp.einsum('bhqk,bkhd->bqhd', p, v)

    qn_b = q_nope.reshape(B, nb, Q_BLOCK, H, -1).transpose(1, 0, 2, 3, 4)
    qr_b = q_rope.reshape(B, nb, Q_BLOCK, H, -1).transpose(1, 0, 2, 3, 4)
    out = lax.map(one_block, (qn_b, qr_b, jnp.arange(nb)))
    return out.transpose(1, 0, 2, 3, 4).reshape(B, S, H, V_HEAD_DIM)


def mla_branch(q_down, kv_down, k_rope_in, cos, sin, q_norm_g, w_uq, kv_norm_g, w_uk, w_uv):
    B, S, _ = q_down.shape
    c_q = rms_norm(q_down, q_norm_g)
    q = (c_q @ w_uq).reshape(B, S, MLA_HEADS, QK_NOPE_DIM + QK_ROPE_DIM)
    q_nope = q[..., :QK_NOPE_DIM]
    q_rope = apply_rope(q[..., QK_NOPE_DIM:], cos[:, :, None, :], sin[:, :, None, :])
    c_kv = rms_norm(kv_down, kv_norm_g)
    k_nope = (c_kv @ w_uk).reshape(B, S, MLA_HEADS, QK_NOPE_DIM)
    v = (c_kv @ w_uv).reshape(B, S, MLA_HEADS, V_HEAD_DIM)
    k_rope = apply_rope(k_rope_in, cos, sin)
    return causal_block_attention(q_nope, q_rope, k_nope, k_rope, v).reshape(B, S, MLA_V_DIM)


def memory_cross_attention(xq, mem_n, w_mem_kv):
    B, S, _ = xq.shape
    kv = mem_n @ w_mem_kv
    k = kv[..., :XATTN_DIM].reshape(B, -1, XATTN_HEADS, XATTN_HEAD_DIM)
    v = kv[..., XATTN_DIM:].reshape(B, -1, XATTN_HEADS, XATTN_HEAD_DIM)
    q = xq.reshape(B, S, XATTN_HEADS, XATTN_HEAD_DIM)
    s = jnp.einsum('bshd,bmhd->bhsm', q, k).astype(jnp.float32) * (XATTN_HEAD_DIM ** -0.5)
    p = jax.nn.softmax(s, axis=-1).astype(v.dtype)
    return jnp.einsum('bhsm,bmhd->bshd', p, v).reshape(B, S, XATTN_DIM)


def hierarchical_moe(h, w_rg, b_rg, w_re, b_re, w_gate_e, w_up_e, w_down_e):
    B, S, D = h.shape
    T = B * S
    xt = h.reshape(T, D)
    g_logits = (xt @ w_rg).astype(jnp.float32) + b_rg.astype(jnp.float32)
    g_probs = jax.nn.softmax(g_logits, axis=-1)
    g_idx = jnp.argmax(g_logits, axis=-1)
    p_g = jnp.take_along_axis(g_probs, g_idx[:, None], axis=1)
    e_logits = ((xt @ w_re).astype(jnp.float32) + b_re.astype(jnp.float32)).reshape(T, N_GROUPS, EXPERTS_PER_GROUP)
    e_sel = jnp.take_along_axis(e_logits, g_idx[:, None, None], axis=1)[:, 0]
    p_e = jax.nn.softmax(e_sel, axis=-1)
    top_p, top_i = lax.top_k(p_e, TOP_K)
    weights = p_g * top_p / jnp.sum(top_p, axis=-1, keepdims=True)
    expert_idx = g_idx[:, None] * EXPERTS_PER_GROUP + top_i

    A = T * TOP_K
    e_flat = expert_idx.reshape(A).astype(jnp.int32)
    tok_flat = jnp.repeat(jnp.arange(T, dtype=jnp.int32), TOP_K)
    w_flat = weights.reshape(A)
    order = jnp.argsort(e_flat)
    e_sorted = e_flat[order]
    counts = jnp.bincount(e_flat, length=N_EXPERTS)
    padded = ((counts + MOE_BLOCK - 1) // MOE_BLOCK) * MOE_BLOCK
    pad_end = jnp.cumsum(padded)
    pad_start = pad_end - padded
    start = jnp.cumsum(counts) - counts
    dest = pad_start[e_sorted] + (jnp.arange(A) - start[e_sorted])
    R = ((A + MOE_BLOCK - 1) // MOE_BLOCK) * MOE_BLOCK + N_EXPERTS * MOE_BLOCK
    n_blk = R // MOE_BLOCK
    row_tok = jnp.full((R,), T, dtype=jnp.int32).at[dest].set(tok_flat[order])
    row_w = jnp.zeros((R,), dtype=jnp.float32).at[dest].set(w_flat[order])
    blk_e = jnp.minimum(jnp.searchsorted(pad_end, jnp.arange(n_blk) * MOE_BLOCK, side='right'), N_EXPERTS - 1)
    x_pad = jnp.concatenate([xt, jnp.zeros((1, D), xt.dtype)], axis=0)

    def run_block(args):
        toks, e = args
        xb = x_pad[toks]
        a = jax.nn.silu(xb @ w_gate_e[e]) * (xb @ w_up_e[e])
        return a @ w_down_e[e]

    yb = lax.map(run_block, (row_tok.reshape(n_blk, MOE_BLOCK), blk_e))
    y = yb.reshape(R, D) * row_w[:, None].astype(yb.dtype)
    out = jax.ops.segment_sum(y, row_tok, num_segments=T + 1)[:T]
    return out.reshape(B, S, D)


def setup_inputs(seed: int = 0) -> dict:
    key = jax.random.key(seed)
    ks = jax.random.split(key, 32)
    f32 = jnp.float32

    def dense(k, shape, fan_in):
        return jax.random.normal(k, shape, f32) * (fan_in ** -0.5)

    def gain(k, shape):
        return 1.0 + 0.05 * jax.random.normal(k, shape, f32)

    L = DEPTH
    return {
        "x": jax.random.normal(ks[0], (BATCH, SEQ, D_MODEL), f32),
        "mem": jax.random.normal(ks[1], (BATCH, MEM_LEN, D_MODEL), f32),
        "positions": (jnp.arange(SEQ, dtype=jnp.int32)[None, :]
                      + jax.random.randint(ks[2], (BATCH, 1), 0, 1024, dtype=jnp.int32)),
        "mix_norm_g": gain(ks[3], (L, D_MODEL)),
        "w_in": dense(ks[4], (L, D_MODEL, D_IN), D_MODEL),
        "gate_b": 0.02 * jax.random.normal(ks[5], (L, N_BRANCHES, D_MODEL), f32),
        "q_norm_g": gain(ks[6], (L, Q_LORA_RANK)),
        "w_uq": dense(ks[7], (L, Q_LORA_RANK, MLA_HEADS * (QK_NOPE_DIM + QK_ROPE_DIM)), Q_LORA_RANK),
        "kv_norm_g": gain(ks[8], (L, KV_LORA_RANK)),
        "w_uk": dense(ks[9], (L, KV_LORA_RANK, MLA_HEADS * QK_NOPE_DIM), KV_LORA_RANK),
        "w_uv": dense(ks[10], (L, KV_LORA_RANK, MLA_V_DIM), KV_LORA_RANK),
        "pool_w": dense(ks[11], (L, POOL_GROUPS, POOL_GROUP_DIM, POOL_GROUP_DIM), POOL_GROUP_DIM),
        "pool_scale": gain(ks[12], (L, POOL_DIM)),
        "mem_norm_g": gain(ks[13], (L, D_MODEL)),
        "w_mem_kv": dense(ks[14], (L, D_MODEL, 2 * XATTN_DIM), D_MODEL),
        "w_br_pool": dense(ks[15], (L, POOL_DIM, D_MODEL), POOL_DIM),
        "w_br_mla": dense(ks[16], (L, MLA_V_DIM, D_MODEL), MLA_V_DIM),
        "w_br_mem": dense(ks[17], (L, XATTN_DIM, D_MODEL), XATTN_DIM),
        "w_out": dense(ks[18], (L, D_MODEL, D_MODEL), D_MODEL),
        "ffn_norm_g": gain(ks[19], (L, D_MODEL)),
        "w_router_group": dense(ks[20], (L, D_MODEL, N_GROUPS), D_MODEL),
        "b_router_group": 0.01 * jax.random.normal(ks[21], (L, N_GROUPS), f32),
        "w_router_expert": dense(ks[22], (L, D_MODEL, N_EXPERTS), D_MODEL),
        "b_router_expert": 0.01 * jax.random.normal(ks[23], (L, N_EXPERTS), f32),
        "w_gate_e": dense(ks[24], (L, N_EXPERTS, D_MODEL, D_EXPERT), D_MODEL),
        "w_up_e": dense(ks[25], (L, N_EXPERTS, D_MODEL, D_EXPERT), D_MODEL),
        "w_down_e": dense(ks[26], (L, N_EXPERTS, D_EXPERT, D_MODEL), D_EXPERT),
        "final_norm_g": gain(ks[27], (D_MODEL,)),
    }


def reference(x, mem, positions, mix_norm_g, w_in, gate_b, q_norm_g, w_uq, kv_norm_g, w_uk, w_uv,
              pool_w, pool_scale, mem_norm_g, w_mem_kv, w_br_pool, w_br_mla, w_br_mem, w_out,
              ffn_norm_g, w_router_group, b_router_group, w_router_expert, b_router_expert,
              w_gate_e, w_up_e, w_down_e, final_norm_g):
    B, S, D = x.shape
    cos, sin = rope_tables(positions)
    split_points = list(np.cumsum(IN_SPLIT_SIZES)[:-1])
    for l in range(DEPTH):
        h = rms_norm(x, mix_norm_g[l])
        proj = h @ w_in[l]
        u_pool, q_down, kv_down, k_rope_in, xq, gate_logits = jnp.split(proj, split_points, axis=-1)
        y_pool = pool_mixer(u_pool, pool_w[l], pool_scale[l])
        y_mla = mla_branch(q_down, kv_down, k_rope_in, cos, sin,
                           q_norm_g[l], w_uq[l], kv_norm_g[l], w_uk[l], w_uv[l])
        y_mem = memory_cross_attention(xq, rms_norm(mem, mem_norm_g[l]), w_mem_kv[l])
        gates = jax.nn.sigmoid(gate_logits.reshape(B, S, N_BRANCHES, D) + gate_b[l])
        merged = (gates[:, :, 0] * (y_pool @ w_br_pool[l])
                  + gates[:, :, 1] * (y_mla @ w_br_mla[l])
                  + gates[:, :, 2] * (y_mem @ w_br_mem[l]))
        x = x + merged @ w_out[l]
        h2 = rms_norm(x, ffn_norm_g[l])
        x = x + hierarchical_moe(h2, w_router_group[l], b_router_group[l], w_router_expert[l],
                                 b_router_expert[l], w_gate_e[l], w_up_e[l], w_down_e[l])
    return rms_norm(x, final_norm_g)
```

```python
import math
from contextlib import ExitStack
from types import SimpleNamespace

import numpy as np
import ml_dtypes

import concourse.bass as bass
import concourse.mybir as mybir
from concourse.bass_utils import run_bass_kernel_spmd

F32 = mybir.dt.float32
BF16 = mybir.dt.bfloat16
I32 = mybir.dt.int32
AF = mybir.ActivationFunctionType
ALU = mybir.AluOpType

D = 1024
DC = 8
MEM_LEN = 256
N_EXP = 32
D_EXP = 256
QLR = 384
KVR = 256
NH = 8
XH = 4
EPS = 1e-6
PI = math.pi
TWO_PI = 2.0 * math.pi


class Prog:
    def __init__(self):
        self.ops = []
        self.barriers = []
        self.enabled = True
        self.stop = None
        self.nobar = set()
        self.excl = set(['psT', 'psTb'] + ['pm%d' % i for i in range(7)] + ['pR0', 'pR1', 'pG0', 'pG1', 'pU0', 'pU1', 'pY0', 'pY1'])

    def mark(self, name):
        if self.stop == name:
            self.enabled = False

    def add(self, eng, fn, r=(), w=(), grp=None, nobar=False):
        if not self.enabled:
            return -1
        w = tuple(w) + tuple(k for k in r if k in self.excl and k not in w)
        self.ops.append((eng, fn, tuple(r), tuple(w), grp))
        if nobar:
            self.nobar.add(len(self.ops) - 1)
        return len(self.ops) - 1

    def barrier(self, pe=False):
        self.barriers.append((len(self.ops), pe))

    def emit(self, nc, stack):
        ops = self.ops
        n = len(ops)
        last_w, readers = {}, {}
        last_on = {}
        last_grp = {}
        bar_snap = {}
        bar_snap_pe = {}
        nbar_pe = 0
        eng_bar_seen = {}
        nbar = 0
        bidx = 0
        bars = self.barriers
        need = [None] * n
        signal = [False] * n
        grp_count = {}
        grp_before = [None] * n
        for i, (eng, fn, r, w, grp) in enumerate(ops):
            while bidx < len(bars) and bars[bidx][0] <= i:
                bar_snap = dict(last_on)
                bar_snap.update({('g', g_): j_ for g_, j_ in last_grp.items()})
                nbar += 1
                if bars[bidx][1]:
                    bar_snap_pe = bar_snap
                    nbar_pe = nbar
                bidx += 1
            d = set()
            for k in r:
                j = last_w.get(k)
                if j is not None:
                    d.add(j)
            for k in w:
                j = last_w.get(k)
                if j is not None:
                    d.add(j)
                for j in readers.get(k, ()):
                    d.add(j)
            if eng == 'pe':
                if eng_bar_seen.get(eng, 0) < nbar_pe:
                    eng_bar_seen[eng] = nbar_pe
                    for j in bar_snap_pe.values():
                        d.add(j)
            elif eng_bar_seen.get(eng, 0) < nbar and i not in self.nobar:
                eng_bar_seen[eng] = nbar
                for j in bar_snap.values():
                    d.add(j)
            for k in r:
                readers.setdefault(k, []).append(i)
            for k in w:
                last_w[k] = i
                readers[k] = []
            last_on[eng] = i
            if grp is not None:
                last_grp[grp] = i
            emax, gset = {}, set()
            for j in d:
                je, _, _, _, jg = ops[j]
                if jg is not None:
                    gset.add(jg)
                else:
                    if je == 'pe' and eng == 'pe':
                        continue
                    if emax.get(je, -1) < j:
                        emax[je] = j
            for j in emax.values():
                signal[j] = True
            need[i] = (emax, {g: grp_count.get(g, 0) for g in gset})
            if grp is not None:
                grp_count[grp] = grp_count.get(grp, 0) + 1
        eng_sem = {e: stack.enter_context(nc.semaphore("sem_" + e)) for e in ('pe', 'act', 'dve', 'pool')}
        grp_sem = {g: stack.enter_context(nc.semaphore("semg_" + str(g))) for g in grp_count}
        cnt = {}
        sig_val = [0] * n
        for i, (eng, fn, r, w, grp) in enumerate(ops):
            if signal[i]:
                cnt[eng] = cnt.get(eng, 0) + 1
                sig_val[i] = cnt[eng]
        per_eng = {}
        for i, o in enumerate(ops):
            per_eng.setdefault(o[0], []).append(i)
        block = stack.enter_context(nc.Block())

        def mk(engname):
            def body(e):
                waited = {}
                for i in per_eng.get(engname, []):
                    _, fn, _, _, grp = ops[i]
                    emax, gneed = need[i]
                    for je, j in emax.items():
                        v = sig_val[j]
                        key = ('e', je)
                        if waited.get(key, 0) < v:
                            e.wait_ge(eng_sem[je], v)
                            waited[key] = v
                    for g, c in gneed.items():
                        v = 16 * c
                        key = ('g', g)
                        if v > 0 and waited.get(key, 0) < v:
                            e.wait_ge(grp_sem[g], v)
                            waited[key] = v
                    ins = fn(e)
                    if ins is None:
                        continue
                    if grp is not None:
                        ins.then_inc(grp_sem[grp], 16)
                    elif signal[i]:
                        ins.then_inc(eng_sem[engname], 1)
            return body

        block.tensor(mk('pe'))
        block.scalar(mk('act'))
        block.vector(mk('dve'))
        block.gpsimd(mk('pool'))
        block.sync(mk('sp'))
        return dict(n_ops=n, counts=cnt)


class Cfg:
    def __init__(self, S):
        self.S = S
        self.NB = S // 128
        self.NGS = S // 512
        self.NG = self.NGS // 2
        self.TO = self.NG * 512
        self.NT = self.TO // 128


def own_groups(cfg, half):
    gs = []
    for i in range(cfg.NGS // 2):
        a, b = 2 * i, 2 * i + 1
        if i % 2 == 0:
            gs.append(a if half == 0 else b)
        else:
            gs.append(b if half == 0 else a)
    return gs


def build_program(cfg, dbg=False, stop=None):
    S, NB, NG, TO, NT = cfg.S, cfg.NB, cfg.NG, cfg.TO, cfg.NT
    nc = bass.Bass("TRN2", target_bir_lowering=False)
    P = Prog()
    P.stop = stop

    def din(name, shape, dt=F32):
        return nc.dram_tensor(name, list(shape), dt, kind="ExternalInput").ap()

    x_seq = din("x_seq", [S, D]); x_own = din("x_own", [TO, D]); x_halo = din("x_halo", [NG * 16, D])
    pos_seq = din("pos_seq", [128, NB], I32); pos_own = din("pos_own", [1, TO], I32)
    mem = din("mem", [MEM_LEN, D])
    invf_col = din("invf_col", [64, 1]); invf_row = din("invf_row", [1, 64])
    inv_cnt = din("inv_cnt", [4, TO])
    masks = din("masks", [NG * 8, 128, 512], BF16)
    ident_d = din("ident", [128, 128], BF16)
    g_mix = din("g_mix", [1, D]); g_mem = din("g_mem", [1, D]); g_ffn = din("g_ffn", [1, D]); g_fin = din("g_fin", [1, D])
    g_kv = din("g_kv", [1, KVR]); g_q = din("g_q", [128, 3]); p_scale = din("p_scale", [128, 4])
    gate_b = din("gate_b", [128, 24]); b_rt = din("b_rt", [1, 36])
    w_pool_in = din("w_in_pool", [D, 512]); w_qd = din("w_in_qd", [D, QLR]); w_kvx = din("w_in_kvx", [D, 384])
    w_xq = din("w_in_xq", [D, 512]); w_gate_in = din("w_in_gate", [D, 3 * D])
    w_uq_n = din("w_uq_n", [QLR, 1024]); w_uq_r = din("w_uq_r", [QLR, 512]); w_uq_rs = din("w_uq_rs", [QLR, 512])
    w_uk = din("w_uk", [KVR, 1024]); w_uv = din("w_uv", [KVR, 1024])
    pool_w = din("pool_w", [4, 128, 128]); w_mem_kv = din("w_mem_kv", [D, 1024])
    w_br_pool = din("w_br_pool", [512, D]); w_br_mla = din("w_br_mla", [1024, D]); w_br_mem = din("w_br_mem", [512, D])
    w_out = din("w_out", [D, D]); w_rt = din("w_rt", [D, 36])
    w_ge = din("w_gate_e", [N_EXP, D, D_EXP]); w_ue = din("w_up_e", [N_EXP, D, D_EXP]); w_de = din("w_down_e", [N_EXP, D_EXP, D])
    out = nc.dram_tensor("out", [TO, D], F32, kind="ExternalOutput").ap()
    x1_d = nc.dram_tensor("x1_scr", [TO, D], F32, kind="Internal").ap()
    dbg_t = {}

    def kview(ap2d):
        return ap2d.rearrange("(c p) n -> p c n", p=128)

    uid = [0]
    with ExitStack() as top:
        def sb(stack, name, shape, dt):
            uid[0] += 1
            return stack.enter_context(nc.sbuf_tensor("s%d_%s" % (uid[0], name), list(shape), dt))

        def ps(stack, name, shape, dt=F32):
            uid[0] += 1
            return stack.enter_context(nc.psum_tensor("p%d_%s" % (uid[0], name), list(shape), dt))

        ident = sb(top, "ident", [128, 128], BF16)
        ones = sb(top, "ones", [128, 128], BF16)
        stat = sb(top, "stat", [128, 64], F32)
        junk = sb(top, "junk", [128, 1024], BF16)
        P.add('sp', lambda e: e.dma_start(out=ident[:], in_=ident_d), w=['ident'], grp='ident')
        P.add('dve', lambda e: e.memset(ones[:], 1.0), w=['ones'])
        stat_i = [0]

        def stat_slot():
            i = stat_i[0] % 16
            stat_i[0] += 1
            return i

        def ld_bcast(stack, name, src_row, n, eng='sp'):
            t = sb(stack, name, [128, n], F32)
            P.add(eng, lambda e: e.dma_start(out=t[:], in_=src_row.partition_broadcast(128)), w=[name], grp=name)
            return t

        def ld_plain(stack, name, src, shape, dt=F32):
            t = sb(stack, name, shape, dt)
            P.add('sp', lambda e: e.dma_start(out=t[:], in_=src), w=[name], grp=name)
            return t

        def ld_cast(stack, name, src_view, shape):
            t = sb(stack, name, shape, BF16)
            P.add('pool', lambda e: e.dma_start(out=t[:], in_=src_view), w=[name], grp=name)
            return t

        def mm(out_ap, lhsT, rhs, start, stop, r, w):
            P.add('pe', lambda e: e.matmul(out_ap, lhsT, rhs, start=start, stop=stop), r=r, w=w)

        def rms_rstd(src_ap, nrows, width, rkeys, psum_src=False):
            s0 = stat_slot()
            k = ('stat', s0)
            c = s0 * 4
            P.add('act', lambda e: e.activation(out=junk[:nrows, :width], in_=src_ap, func=AF.Square,
                                                scale=1.0 / math.sqrt(width), accum_out=stat[:nrows, c:c + 1]),
                  r=rkeys, w=['junk', k])
            P.add('act', lambda e: e.activation(out=stat[:nrows, c + 1:c + 2], in_=stat[:nrows, c:c + 1], func=AF.Ln,
                                                bias=EPS, scale=1.0), r=[k], w=[k])
            P.add('act', lambda e: e.activation(out=stat[:nrows, c + 2:c + 3], in_=stat[:nrows, c + 1:c + 2], func=AF.Exp,
                                                scale=-0.5), r=[k], w=[k])
            return stat[:nrows, c + 2:c + 3], k

        MAGIC = 12582912.0
        C1 = 6.28125
        C2 = TWO_PI - 6.28125

        def range_reduce(buf, tmp, bk, tk):
            P.add('dve', lambda e: e.tensor_scalar(out=tmp, in0=buf, scalar1=1.0 / TWO_PI, scalar2=MAGIC, op0=ALU.mult, op1=ALU.add),
                  r=[bk], w=[tk])
            P.add('dve', lambda e: e.tensor_scalar(out=tmp, in0=tmp, scalar1=MAGIC, scalar2=None, op0=ALU.subtract), r=[tk], w=[tk])
            P.add('dve', lambda e: e.scalar_tensor_tensor(out=buf, in0=tmp, scalar=-C1, in1=buf, op0=ALU.mult, op1=ALU.add), r=[tk, bk], w=[bk])
            P.add('dve', lambda e: e.scalar_tensor_tensor(out=buf, in0=tmp, scalar=-C2, in1=buf, op0=ALU.mult, op1=ALU.add), r=[tk, bk], w=[bk])
            P.add('dve', lambda e: e.tensor_scalar(out=buf, in0=buf, scalar1=-PI, scalar2=PI, op0=ALU.max, op1=ALU.min), r=[bk], w=[bk])

        with ExitStack() as A:
            kvT = sb(A, "kvT", [128, 3, S], BF16)
            ckvT = kvT
            ckv_tok = sb(A, "ckv_tok", [128, NB, KVR], BF16)
            gmix_b = ld_bcast(A, "gmix_b", g_mix, D)
            gkv_b = ld_bcast(A, "gkv_b", g_kv, KVR)
            invf_b = ld_bcast(A, "invf_b", invf_row, 64)
            invf_c = ld_plain(A, "invf_c", invf_col, [64, 1])
            gq_t = ld_plain(A, "gq_t", g_q, [128, 3])
            psc_t = ld_plain(A, "psc_t", p_scale, [128, 4])
            gb_t = ld_plain(A, "gb_t", gate_b, [128, 24])
            w_uv_t = ld_cast(A, "w_uv_t", kview(w_uv), [128, 2, 1024])
            poolw_t = ld_cast(A, "poolw_t", pool_w.rearrange("g c d -> c g d"), [128, 4, 128])
            w_ukT = sb(A, "w_ukT", [128, NH, KVR], BF16)
            KmemT = sb(A, "KmemT", [128, XH, MEM_LEN], BF16)
            Vmem = sb(A, "Vmem", [128, 2, 512], BF16)
            xr = [sb(A, "xr%d" % i, [128, D], F32) for i in range(2)]
            hn = [sb(A, "hn%d" % i, [128, D], BF16) for i in range(2)]
            NRING = 4
            ring = [sb(A, "ring%d" % i, [128, 4096], BF16) for i in range(NRING)]
            ring_i = [0]
            xr_i = [0]

            def wload(src2d, kc, ncols):
                s = ring_i[0] % NRING
                ring_i[0] += 1
                view = ring[s][:, 0:kc * ncols].rearrange("p (c n) -> p c n", c=kc)
                P.add('pool', lambda e: e.dma_start(out=view, in_=kview(src2d)), w=[('ring', s)], grp=('ring', s), nobar=True)
                return view, ('ring', s)

            with ExitStack() as PSA:
                psT = ps(PSA, "psT", [128, 1024], BF16)
                pm = [ps(PSA, "pm%d" % i, [128, 512]) for i in range(7)]

                def norm_tile_to_hT(src_rows, nrows, gb, gbk, hT_dst_fn):
                    s = xr_i[0] % 2
                    xr_i[0] += 1
                    xt, ht = xr[s], hn[s]
                    P.add('sp', lambda e: e.dma_start(out=xt[:nrows, :], in_=src_rows), w=[('xr', s)], grp=('xr', s), nobar=True)
                    rstd, k = rms_rstd(xt[:nrows, :], nrows, D, [('xr', s)])
                    P.add('dve', lambda e: e.scalar_tensor_tensor(out=ht[:nrows, :], in0=xt[:nrows, :], scalar=rstd,
                                                                   in1=gb[:nrows, :], op0=ALU.mult, op1=ALU.mult),
                          r=[('xr', s), k, gbk], w=[('hn', s)])
                    for c in range(DC):
                        P.add('pe', lambda e, c=c: e.transpose(out=psT[:, c * 128:c * 128 + nrows],
                                                               in_=ht[:nrows, c * 128:(c + 1) * 128],
                                                               identity=ident[:nrows, :nrows]),
                              r=[('hn', s), 'ident'], w=['psT'])
                    hT_dst_fn(psT[:, :].rearrange("p (c t) -> p c t", c=DC)[:, :, :nrows])

                def norm_a(src_rows, nrows, gb, gbk):
                    s = xr_i[0] % 2
                    xr_i[0] += 1
                    xt, ht = xr[s], hn[s]
                    P.add('sp', lambda e: e.dma_start(out=xt[:nrows, :], in_=src_rows), w=[('xr', s)], grp=('xr', s), nobar=True)
                    rstd, k = rms_rstd(xt[:nrows, :], nrows, D, [('xr', s)])
                    P.add('dve', lambda e: e.scalar_tensor_tensor(out=ht[:nrows, :], in0=xt[:nrows, :], scalar=rstd,
                                                                   in1=gb[:nrows, :], op0=ALU.mult, op1=ALU.mult),
                          r=[('xr', s), k, gbk], w=[('hn', s)])
                    return s, nrows

                def norm_b(handle, hT_dst_fn):
                    s, nrows = handle
                    ht = hn[s]
                    for c in range(DC):
                        P.add('pe', lambda e, c=c: e.transpose(out=psT[:, c * 128:c * 128 + nrows],
                                                               in_=ht[:nrows, c * 128:(c + 1) * 128],
                                                               identity=ident[:nrows, :nrows]),
                              r=[('hn', s), 'ident'], w=['psT'])
                    hT_dst_fn(psT[:, :].rearrange("p (c t) -> p c t", c=DC)[:, :, :nrows])

                P.mark('A0a')
                with ExitStack() as AU:
                    w_uk_t = ld_cast(AU, "w_uk_t", kview(w_uk), [128, 2, 1024])
                    for h in range(NH):
                        for j in range(2):
                            P.add('pe', lambda e, h=h, j=j: e.transpose(out=psT[:, j * 128:(j + 1) * 128],
                                                                        in_=w_uk_t[:, j, h * 128:(h + 1) * 128], identity=ident[:]),
                                  r=['w_uk_t', 'ident'], w=['psT'])
                        P.add('dve', lambda e, h=h: e.tensor_copy(out=w_ukT[:, h, :], in_=psT[:, 0:256]), r=['psT'], w=['w_ukT'])
                    P.barrier()
                P.mark('A0b')
                with ExitStack() as A0:
                    gmem_b = ld_bcast(A0, "gmem_b", g_mem, D)
                    memT = sb(A0, "memT", [128, DC, MEM_LEN], BF16)
                    for mt in range(2):
                        def dst(src, mt=mt):
                            P.add('dve', lambda e: e.tensor_copy(out=memT[:, :, mt * 128:(mt + 1) * 128], in_=src),
                                  r=['psT'], w=['memT'])
                        norm_tile_to_hT(mem[mt * 128:(mt + 1) * 128, :], 128, gmem_b, 'gmem_b', dst)
                    P.mark('A0c')
                    for half in range(2):
                        wv, wk = wload(w_mem_kv[:, half * 512:(half + 1) * 512], DC, 512)
                        if half == 0:
                            for hh in range(XH):
                                for c in range(DC):
                                    mm(pm[0][:, 0:MEM_LEN], wv[:, c, hh * 128:(hh + 1) * 128], memT[:, c, :], c == 0, c == DC - 1,
                                       [wk, 'memT'], ['pm0'])
                                P.add('dve', lambda e, hh=hh: e.tensor_copy(out=KmemT[:, hh, :], in_=pm[0][:, 0:MEM_LEN]),
                                      r=['pm0'], w=['KmemT'])
                        else:
                            P.mark('A0d')
                            for mt in range(2):
                                for c in range(DC):
                                    mm(pm[1][:, :], memT[:, c, mt * 128:(mt + 1) * 128], wv[:, c, :], c == 0, c == DC - 1,
                                       [wk, 'memT'], ['pm1'])
                                P.add('act', lambda e, mt=mt: e.activation(out=Vmem[:, mt, :], in_=pm[1][:, :], func=AF.Identity), r=['pm1'], w=['Vmem'])
                    P.barrier()
                P.mark('A0')

                with ExitStack() as A1:
                    w_kvx_t = ld_cast(A1, "w_kvx_t", kview(w_kvx), [128, DC, 384])
                    hTt = sb(A1, "hTt", [128, DC, 128], BF16)
                    kro = sb(A1, "kro", [128, 128], BF16)
                    P.add('dve', lambda e: e.memset(kro[:], 0.0), w=[('kro', 0)])
                    tA = sb(A1, "tA", [128, 64], F32)
                    tB = sb(A1, "tB", [128, 64], F32)
                    posi = sb(A1, "posi", [128, NB], I32)
                    posf = sb(A1, "posf", [128, NB], F32)
                    angc = sb(A1, "angc", [128, NB, 64], F32)
                    angs = sb(A1, "angs", [128, NB, 64], F32)
                    P.add('sp', lambda e: e.dma_start(out=posi[:], in_=pos_seq),
                          w=['posi'], grp='posi')
                    P.add('dve', lambda e: e.tensor_copy(out=posf[:], in_=posi[:]), r=['posi'], w=['posf'])
                    for t in range(NB):
                        P.add('dve', lambda e, t=t: e.tensor_scalar(out=angc[:, t, :], in0=invf_b[:, :], scalar1=posf[:, t:t + 1],
                                                                     scalar2=PI / 2, op0=ALU.mult, op1=ALU.add),
                              r=['posf', 'invf_b'], w=['angc'])
                        P.add('dve', lambda e, t=t: e.tensor_scalar(out=angs[:, t, :], in0=invf_b[:, :], scalar1=posf[:, t:t + 1],
                                                                     scalar2=0.0, op0=ALU.mult, op1=ALU.add),
                              r=['posf', 'invf_b'], w=['angs'])
                    angt = sb(A1, "angt", [128, NB, 64], F32)
                    range_reduce(angc[:], angt[:], 'angc', 'angt')
                    range_reduce(angs[:], angt[:], 'angs', 'angt')
                    P.add('act', lambda e: e.activation(out=angc[:], in_=angc[:], func=AF.Sin, scale=1.0),
                          r=['angc'], w=['angc'])
                    P.add('act', lambda e: e.activation(out=angs[:, :, 0:32], in_=angs[:, :, 0:32], func=AF.Sin, scale=-1.0),
                          r=['angs'], w=['angs'])
                    P.add('act', lambda e: e.activation(out=angs[:, :, 32:64], in_=angs[:, :, 32:64], func=AF.Sin, scale=1.0),
                          r=['angs'], w=['angs'])
                    P.mark('A1a')
                    hTt2 = [hTt, sb(A1, "hTt_b", [128, DC, 128], BF16)]
                    kro2 = [kro, sb(A1, "kro_b", [128, 128], BF16)]
                    P.add('dve', lambda e: e.memset(kro2[1][:], 0.0), w=[('kro', 1)])
                    tA2 = [tA, sb(A1, "tA_b", [128, 64], F32)]
                    tB2 = [tB, sb(A1, "tB_b", [128, 64], F32)]

                    psT2 = pm[6][:, :].bitcast(BF16)

                    def front_a(t):
                        s = t % 2
                        xt, ht = xr[s], hn[s]
                        P.add('sp', lambda e: e.dma_start(out=xt[:, :], in_=x_seq[t * 128:(t + 1) * 128, :]), w=[('xr', s)], grp=('xr', s),
                              nobar=True)
                        rstd, k = rms_rstd(xt[:, :], 128, D, [('xr', s)])
                        P.add('dve', lambda e: e.scalar_tensor_tensor(out=ht[:, :], in0=xt[:, :], scalar=rstd, in1=gmix_b[:, :],
                                                                       op0=ALU.mult, op1=ALU.mult),
                              r=[('xr', s), k, 'gmix_b'], w=[('hn', s)])

                    def front_b(t):
                        s = t % 2
                        ht = hn[s]
                        hb, hk = hTt2[t % 2], ('hTt', t % 2)
                        for c in range(DC):
                            P.add('pe', lambda e, c=c: e.transpose(out=psT[:, c * 128:(c + 1) * 128], in_=ht[:, c * 128:(c + 1) * 128],
                                                                   identity=ident[:]), r=[('hn', s), 'ident'], w=['psT'])
                        P.add('dve', lambda e: e.tensor_copy(out=hb[:], in_=psT[:, :].rearrange("p (c t) -> p c t", c=DC)), r=['psT'], w=[hk])
                        pk, pkk = pm[t % 2], 'pm%d' % (t % 2)
                        for c in range(DC):
                            mm(pk[:, 0:384], hb[:, c, :], w_kvx_t[:, c, :], c == 0, c == DC - 1, [hk, 'w_kvx_t'], [pkk])

                    def back(t):
                        pk, pkk = pm[t % 2], 'pm%d' % (t % 2)
                        ta, tb, kr = tA2[t % 2], tB2[t % 2], kro2[t % 2]
                        tak, tbk, krk = ('tA', t % 2), ('tB', t % 2), ('kro', t % 2)
                        rstd, k = rms_rstd(pk[:, 0:KVR], 128, KVR, [pkk])
                        P.add('dve', lambda e: e.scalar_tensor_tensor(
                            out=ckv_tok[:, t, :], in0=pk[:, 0:KVR], scalar=rstd, in1=gkv_b[:, :], op0=ALU.mult, op1=ALU.mult),
                            r=[pkk, k, 'gkv_b'], w=[('ckv_tok', t)])
                        P.add('dve', lambda e: e.tensor_tensor(out=ta[:], in0=pk[:, 256:320], in1=angc[:, t, :], op=ALU.mult),
                              r=[pkk, 'angc'], w=[tak])
                        P.add('dve', lambda e: e.tensor_tensor(out=tb[:], in0=pk[:, 320:384], in1=angs[:, t, :], op=ALU.mult),
                              r=[pkk, 'angs'], w=[tbk])
                        P.add('dve', lambda e: e.tensor_tensor(out=kr[:, 0:64], in0=ta[:], in1=tb[:], op=ALU.add), r=[tak, tbk], w=[krk])
                        for j in range(2):
                            P.add('pe', lambda e, j=j: e.transpose(out=psT2[:, j * 128:(j + 1) * 128],
                                                                   in_=ckv_tok[:, t, j * 128:(j + 1) * 128], identity=ident[:]),
                                  r=[('ckv_tok', t), 'ident'], w=['pm6'])
                        P.add('pe', lambda e: e.transpose(out=psT2[:, 256:384], in_=kr[:, :], identity=ident[:]),
                              r=[krk, 'ident'], w=['pm6'])
                        P.add('dve', lambda e: e.tensor_copy(out=kvT[:, :, t * 128:(t + 1) * 128],
                                                             in_=psT2[:, 0:384].rearrange("p (j t) -> p j t", j=3)),
                              r=['pm6'], w=[('ckvT', t), ('kropeT', t)])

                    front_a(0)
                    front_a(1)
                    front_b(0)
                    for t in range(NB):
                        if t + 1 < NB:
                            front_b(t + 1)
                        if t + 2 < NB:
                            front_a(t + 2)
                        back(t)
                    xr_i[0] = 0
                    P.barrier()
                P.mark('A1')
                if dbg:
                    for nm, t_, shp, dt in (("d_ckvT", ckvT, [128, 3, S], BF16),
                                            ("d_KmemT", KmemT, [128, XH, MEM_LEN], BF16), ("d_Vmem", Vmem, [128, 2, 512], BF16)):
                        dd = nc.dram_tensor(nm, shp, dt, kind="ExternalOutput").ap()
                        P.add('sp', lambda e, dd=dd, t_=t_: e.dma_start(out=dd, in_=t_[:]),
                              r=[('ckvT', t) for t in range(NB)] + [('kropeT', t) for t in range(NB)] + ['KmemT', 'Vmem'],
                              w=['outdone'], grp='out')
                BUILD_A2(SimpleNamespace(**locals()))
        BUILD_B(SimpleNamespace(**locals()))
        P.add('sp', lambda e: None, r=['outdone'])
        info = P.emit(nc, top)
    return nc, info


def BUILD_A2(ns):
    P, nc, cfg = ns.P, ns.nc, ns.cfg
    S, NB, NG, TO, NT = cfg.S, cfg.NB, cfg.NG, cfg.TO, cfg.NT
    sb, mm, wload, rms_rstd, norm_tile_to_hT = ns.sb, ns.mm, ns.wload, ns.rms_rstd, ns.norm_tile_to_hT
    pm, psT, ident, ones = ns.pm, ns.psT, ns.ident, ns.ones
    ckvT, ckv_tok = ns.ckvT, ns.ckv_tok
    xr, xr_i = ns.xr, ns.xr_i
    SC_MLA = 1.0 / math.sqrt(192.0)
    SC_MEM = 1.0 / math.sqrt(128.0)
    pmk = ['pm%d' % i for i in range(7)]
    rot = [0]

    def bank():
        b = rot[0] % 7
        rot[0] += 1
        return pm[b], pmk[b]

    def do_group(G):
        kmax = 8 * (G + 1)
        with ExitStack() as GS:
            hT = sb(GS, "hT", [128, DC, 528], BF16)
            xqT = sb(GS, "xqT", [128, XH, 512], BF16)
            cqT = sb(GS, "cqT", [128, 3, 512], BF16)
            cos2 = sb(GS, "cos2", [64, 512], F32)
            sin2s = sb(GS, "sin2s", [64, 512], F32)
            qabs = sb(GS, "qabs", [128, NH, 2, 512], BF16)
            qr = sb(GS, "qr", [128, NH, 512], BF16)
            P.add('dve', lambda e: e.memset(qr[64:128, :, :], 0.0), w=[('qr', h_) for h_ in range(NH)])
            PT = [sb(GS, "PT%d" % i, [128, 512], BF16) for i in range(3)]
            mk = [sb(GS, "mk%d" % i, [128, 512], BF16) for i in range(2)]
            rs = sb(GS, "rs", [128, 512], F32)
            olat = sb(GS, "olat", [128, 2, 512], BF16)
            ocp = sb(GS, "ocp", [128, 2, 512], F32)
            ymlaT = sb(GS, "ymlaT", [128, NH, 512], BF16)
            ymemT = sb(GS, "ymemT", [128, XH, 512], BF16)
            ypoolT = sb(GS, "ypoolT", [128, 4, 512], BF16)
            PTm = sb(GS, "PTm", [128, 2, 512], BF16)

            def dst_h(src):
                P.add('act', lambda e: e.activation(out=hT[:, :, 0:16], in_=src, func=AF.Identity), r=['psT'], w=['hT'])

            def mk_dst(t):
                def dst_t(src):
                    P.add('act', lambda e: e.activation(out=hT[:, :, 16 + t * 128:16 + (t + 1) * 128], in_=src, func=AF.Identity),
                          r=['psT'], w=['hT'])
                return dst_t
            jobs = [(ns.x_halo[G * 16:(G + 1) * 16, :], 16, dst_h)]
            for t in range(4):
                r0 = (G * 4 + t) * 128
                jobs.append((ns.x_own[r0:r0 + 128, :], 128, mk_dst(t)))
            hs = {}
            hs[0] = ns.norm_a(jobs[0][0], jobs[0][1], ns.gmix_b, 'gmix_b')
            hs[1] = ns.norm_a(jobs[1][0], jobs[1][1], ns.gmix_b, 'gmix_b')
            for i_ in range(5):
                ns.norm_b(hs[i_], jobs[i_][2])
                if i_ + 2 < 5:
                    hs[i_ + 2] = ns.norm_a(jobs[i_ + 2][0], jobs[i_ + 2][1], ns.gmix_b, 'gmix_b')

            with ExitStack() as E:
                u = [sb(E, "u%d" % i, [128, 528], F32) for i in range(2)]
                sAB = [sb(E, "sA", [128, 528], F32), sb(E, "sB", [128, 528], F32)]
                invc = [sb(E, "invc%d" % i, [128, 512], F32) for i in range(2)]
                tmpp = sb(E, "tmpp", [128, 512], F32)
                pT = [sb(E, "pT%d" % i, [128, 512], BF16) for i in range(2)]
                qdT = sb(E, "qdT", [128, 3, 512], F32)
                sq = sb(E, "sq", [128, 3, 512], BF16)
                lnq = sb(E, "lnq", [128, 512], F32)
                rstdq = lnq
                wv, wk = wload(ns.w_pool_in, DC, 512)
                for g in range(4):
                    ug, uk = u[g % 2], ('u', g % 2)
                    pb, pbk = bank()
                    for c in range(DC):
                        mm(pb[:, :], wv[:, c, g * 128:(g + 1) * 128], hT[:, c, 16:528], c == 0, c == DC - 1, [wk, 'hT'], [pbk])
                    P.add('act', lambda e, ug=ug, pb=pb: e.activation(out=ug[:, 16:528], in_=pb[:, :], func=AF.Identity), r=[pbk], w=[uk])
                    ph, phk = bank()
                    for c in range(DC):
                        mm(ph[:, 0:16], wv[:, c, g * 128:(g + 1) * 128], hT[:, c, 0:16], c == 0, c == DC - 1, [wk, 'hT'], [phk])
                    P.add('dve', lambda e, ug=ug, ph=ph: e.tensor_copy(out=ug[:, 0:16], in_=ph[:, 0:16]), r=[phk], w=[uk])
                    src, srck = ug, uk
                    for k in range(g + 1):
                        sh = 1 << k
                        lo = 2 * sh - 1
                        dstb, dstk = sAB[k % 2], ('sAB', k % 2)
                        P.add('dve', lambda e, dstb=dstb, src=src, lo=lo, sh=sh: e.tensor_tensor(
                            out=dstb[:, lo:528], in0=src[:, lo:528], in1=src[:, lo - sh:528 - sh], op=ALU.add),
                            r=[srck], w=[dstk])
                        src, srck = dstb, dstk
                    ic, ick = invc[g % 2], ('invc', g % 2)
                    P.add('sp', lambda e, ic=ic, g=g: e.dma_start(
                        out=ic[:], in_=ns.inv_cnt[g:g + 1, G * 512:(G + 1) * 512].partition_broadcast(128)), w=[ick], grp=ick)
                    P.add('dve', lambda e, src=src, ic=ic: e.tensor_tensor(out=tmpp[:], in0=src[:, 16:528], in1=ic[:], op=ALU.mult),
                          r=[srck, ick], w=['tmpp'])
                    pTg, pTk = pT[g % 2], ('pT', g % 2)
                    P.add('dve', lambda e, pTg=pTg, ug=ug: e.tensor_tensor(out=pTg[:], in0=tmpp[:], in1=ug[:, 16:528], op=ALU.subtract),
                          r=['tmpp', uk], w=[pTk])
                    py, pyk = bank()
                    mm(py[:, :], ns.poolw_t[:, g, :], pTg[:, :], True, True, ['poolw_t', pTk], [pyk])
                    P.add('dve', lambda e, g=g, py=py: e.tensor_scalar(out=ypoolT[:, g, :], in0=py[:, :], scalar1=ns.psc_t[:, g:g + 1],
                                                                        scalar2=None, op0=ALU.mult), r=[pyk, 'psc_t'], w=['ypoolT'])
                wv, wk = wload(ns.w_qd, DC, QLR)
                for oc in range(3):
                    pb, pbk = bank()
                    for c in range(DC):
                        mm(pb[:, :], wv[:, c, oc * 128:(oc + 1) * 128], hT[:, c, 16:528], c == 0, c == DC - 1, [wk, 'hT'], [pbk])
                    P.add('dve', lambda e, oc=oc, pb=pb: e.tensor_copy(out=qdT[:, oc, :], in_=pb[:, :]), r=[pbk], w=[('qdT', oc)])
                    P.add('act', lambda e, oc=oc, pb=pb: e.activation(out=sq[:, oc, :], in_=pb[:, :], func=AF.Square), r=[pbk], w=[('sq', oc)])
                pz, pzk = bank()
                for oc in range(3):
                    mm(pz[:, :], ones[:, :], sq[:, oc, :], oc == 0, oc == 2, ['ones', ('sq', oc)], [pzk])
                P.add('act', lambda e, pz=pz: e.activation(out=lnq[:], in_=pz[:, :], func=AF.Ln, bias=EPS, scale=1.0 / QLR), r=[pzk], w=['lnq'])
                P.add('act', lambda e: e.activation(out=lnq[:], in_=lnq[:], func=AF.Exp, scale=-0.5), r=['lnq'], w=['lnq', 'rstdq'])
                for oc in range(3):
                    P.add('dve', lambda e, oc=oc: e.scalar_tensor_tensor(out=cqT[:, oc, :], in0=qdT[:, oc, :], scalar=ns.gq_t[:, oc:oc + 1],
                                                                         in1=rstdq[:], op0=ALU.mult, op1=ALU.mult),
                          r=[('qdT', oc), 'gq_t', 'rstdq'], w=['cqT'])
                wv, wk = wload(ns.w_xq, DC, 512)
                for oc in range(XH):
                    pb, pbk = bank()
                    for c in range(DC):
                        mm(pb[:, :], wv[:, c, oc * 128:(oc + 1) * 128], hT[:, c, 16:528], c == 0, c == DC - 1, [wk, 'hT'], [pbk])
                    P.add('act', lambda e, oc=oc, pb=pb: e.activation(out=xqT[:, oc, :], in_=pb[:, :], func=AF.Identity), r=[pbk], w=['xqT'])
                P.barrier()
            P.mark('A2e')

            with ExitStack() as M:
                qn = sb(M, "qn", [128, NH, 512], BF16)
                t1 = sb(M, "t1", [64, 512], F32)
                t2 = sb(M, "t2", [64, 512], F32)
                posb = sb(M, "posb", [64, 512], I32)
                P.add('sp', lambda e: e.dma_start(out=posb[:], in_=ns.pos_own[0:1, G * 512:(G + 1) * 512].partition_broadcast(64)),
                      w=['posb'], grp='posb')
                P.add('dve', lambda e: e.tensor_copy(out=t1[:], in_=posb[:]), r=['posb'], w=['t1'])
                P.add('dve', lambda e: e.tensor_scalar(out=cos2[:], in0=t1[:], scalar1=ns.invf_c[:, 0:1], scalar2=PI / 2,
                                                        op0=ALU.mult, op1=ALU.add), r=['t1', 'invf_c'], w=['cos2'])
                P.add('dve', lambda e: e.tensor_scalar(out=sin2s[:], in0=t1[:], scalar1=ns.invf_c[:, 0:1], scalar2=0.0,
                                                        op0=ALU.mult, op1=ALU.add), r=['t1', 'invf_c'], w=['sin2s'])
                ns.range_reduce(cos2[:], t2[:], 'cos2', 't2')
                ns.range_reduce(sin2s[:], t2[:], 'sin2s', 't2')
                P.add('act', lambda e: e.activation(out=cos2[:], in_=cos2[:], func=AF.Sin, scale=1.0), r=['cos2'], w=['cos2'])
                P.add('act', lambda e: e.activation(out=sin2s[0:32, :], in_=sin2s[0:32, :], func=AF.Sin, scale=-1.0),
                      r=['sin2s'], w=['sin2s'])
                P.add('act', lambda e: e.activation(out=sin2s[32:64, :], in_=sin2s[32:64, :], func=AF.Sin, scale=1.0),
                      r=['sin2s'], w=['sin2s'])
                for hh in range(XH):
                    sbk = []
                    for mb in range(2):
                        pb, pbk = bank()
                        mm(pb[:, :], ns.KmemT[:, hh, mb * 128:(mb + 1) * 128], xqT[:, hh, :], True, True, ['KmemT', 'xqT'], [pbk])
                        P.add('act', lambda e, mb=mb, pb=pb: e.activation(out=PTm[:, mb, :], in_=pb[:, :], func=AF.Exp, scale=SC_MEM),
                              r=[pbk], w=[('PTm', mb)])
                    po, pok = bank()
                    pz, pzk = bank()
                    for mb in range(2):
                        mm(po[:, :], ns.Vmem[:, mb, hh * 128:(hh + 1) * 128], PTm[:, mb, :], mb == 0, mb == 1, ['Vmem', ('PTm', mb)], [pok])
                    for mb in range(2):
                        mm(pz[:, :], ones[:, :], PTm[:, mb, :], mb == 0, mb == 1, ['ones', ('PTm', mb)], [pzk])
                    P.add('dve', lambda e, pz=pz: e.reciprocal(out=rs[:], in_=pz[:, :]), r=[pzk], w=['rs'])
                    P.add('dve', lambda e, hh=hh, po=po: e.tensor_tensor(out=ymemT[:, hh, :], in0=po[:, :], in1=rs[:], op=ALU.mult),
                          r=[pok, 'rs'], w=['ymemT'])
                wn, wnk = wload(ns.w_uq_n, 3, 1024)
                wr, wrk = wload(ns.w_uq_r, 3, 512)
                wrs, wrsk = wload(ns.w_uq_rs, 3, 512)
                for h in range(NH):
                    pb, pbk = bank()
                    for c in range(3):
                        mm(pb[:, :], wn[:, c, h * 128:(h + 1) * 128], cqT[:, c, :], c == 0, c == 2, [wnk, 'cqT'], [pbk])
                    P.add('act', lambda e, h=h, pb=pb: e.activation(out=qn[:, h, :], in_=pb[:, :], func=AF.Identity), r=[pbk], w=[('qn', h)])
                for h in range(NH):
                    for j in range(2):
                        pb, pbk = bank()
                        mm(pb[:, :], ns.w_ukT[:, h, j * 128:(j + 1) * 128], qn[:, h, :], True, True, ['w_ukT', ('qn', h)], [pbk])
                        if j == 0:
                            P.add('act', lambda e, h=h, j=j, pb=pb: e.activation(out=qabs[:, h, j, :], in_=pb[:, :], func=AF.Identity), r=[pbk], w=[('qabs', h)])
                        else:
                            P.add('dve', lambda e, h=h, j=j, pb=pb: e.tensor_copy(out=qabs[:, h, j, :], in_=pb[:, :]), r=[pbk], w=[('qabs', h)])
                for h in range(NH):
                    pa, pak = bank()
                    for c in range(3):
                        mm(pa[0:64, :], wr[:, c, h * 64:(h + 1) * 64], cqT[:, c, :], c == 0, c == 2, [wrk, 'cqT'], [pak])
                    P.add('dve', lambda e, pa=pa: e.tensor_tensor(out=t1[:], in0=pa[0:64, :], in1=cos2[:], op=ALU.mult), r=[pak, 'cos2'], w=['t1'])
                    pb, pbk = bank()
                    for c in range(3):
                        mm(pb[0:64, :], wrs[:, c, h * 64:(h + 1) * 64], cqT[:, c, :], c == 0, c == 2, [wrsk, 'cqT'], [pbk])
                    P.add('dve', lambda e, pb=pb: e.tensor_tensor(out=t2[:], in0=pb[0:64, :], in1=sin2s[:], op=ALU.mult), r=[pbk, 'sin2s'], w=['t2'])
                    P.add('dve', lambda e, h=h: e.tensor_tensor(out=qr[0:64, h, :], in0=t1[:], in1=t2[:], op=ALU.add), r=['t1', 't2'], w=[('qr', h)])
                P.barrier()

            P.mark('A2m')
            cells = [(h, kb) for h in range(NH) for kb in range(kmax)]
            SB = [(pm[0], 'pm0'), (pm[1], 'pm1'), (pm[2], 'pm2')]
            O0, O1, Z = pm[3], pm[4], pm[5]
            deferred = []
            mki = [0]

            def emit_qk(i):
                h, kb = cells[i]
                sbk_, sk = SB[i % 3]
                ks = slice(kb * 128, (kb + 1) * 128)
                mm(sbk_[:, :], ckvT[:, 0, ks], qabs[:, h, 0, :], True, False, [('ckvT', kb), ('qabs', h)], [sk])
                mm(sbk_[:, :], ckvT[:, 1, ks], qabs[:, h, 1, :], False, False, [('ckvT', kb), ('qabs', h)], [sk])
                mm(sbk_[:, :], ckvT[:, 2, ks], qr[:, h, :], False, True, [('kropeT', kb), ('qr', h)], [sk])

            def emit_rest(i):
                h, kb = cells[i]
                sbk_, sk = SB[i % 3]
                pt, ptk = PT[i % 3], ('PT', i % 3)
                P.add('act', lambda e: e.activation(out=pt[:], in_=sbk_[:, :], func=AF.Exp, scale=SC_MLA), r=[sk], w=[ptk])
                if kb >= kmax - 8:
                    m, mkk = mk[mki[0] % 2], ('mk', mki[0] % 2)
                    mki[0] += 1
                    mi = G * 8 + (kb - (kmax - 8))
                    P.add('sp', lambda e: e.dma_start(out=m[:], in_=ns.masks[mi]), w=[mkk], grp=mkk)
                    P.add('dve', lambda e: e.tensor_tensor(out=pt[:], in0=pt[:], in1=m[:], op=ALU.mult), r=[ptk, mkk], w=[ptk])
                first, last = kb == 0, kb == kmax - 1
                mm(O0[:, :], ckv_tok[:, kb, 0:128], pt[:], first, last, [('ckv_tok', kb), ptk], ['pm3'])
                mm(O1[:, :], ckv_tok[:, kb, 128:256], pt[:], first, last, [('ckv_tok', kb), ptk], ['pm4'])
                mm(Z[:, :], ones[:, :], pt[:], first, last, ['ones', ptk], ['pm5'])
                if last:
                    P.add('act', lambda e: e.activation(out=ocp[:, 0, :], in_=O0[:, :], func=AF.Identity), r=['pm3'], w=[('ocp', 0)])
                    P.add('dve', lambda e: e.tensor_copy(out=ocp[:, 1, :], in_=O1[:, :]), r=['pm4'], w=[('ocp', 1)])
                    P.add('dve', lambda e: e.reciprocal(out=rs[:], in_=Z[:, :]), r=['pm5'], w=['rs'])
                    P.add('dve', lambda e: e.tensor_tensor(out=olat[:, 0, :], in0=ocp[:, 0, :], in1=rs[:], op=ALU.mult), r=[('ocp', 0), 'rs'], w=['olat'])
                    P.add('dve', lambda e: e.tensor_tensor(out=olat[:, 1, :], in0=ocp[:, 1, :], in1=rs[:], op=ALU.mult), r=[('ocp', 1), 'rs'], w=['olat'])
                    deferred.append((h, i + 3))

            def flush(i, force=False):
                while deferred and (force or deferred[0][1] <= i):
                    h, _ = deferred.pop(0)
                    for j in range(2):
                        mm(pm[6][:, :], ns.w_uv_t[:, j, h * 128:(h + 1) * 128], olat[:, j, :], j == 0, j == 1, ['w_uv_t', 'olat'], ['pm6'])
                    P.add('act', lambda e, h=h: e.activation(out=ymlaT[:, h, :], in_=pm[6][:, :], func=AF.Identity), r=['pm6'], w=['ymlaT'])

            nc_ = len(cells)
            for i in range(nc_ + 2):
                if i < nc_:
                    emit_qk(i)
                if i >= 2:
                    emit_rest(i - 2)
                    flush(i - 2)
            flush(0, force=True)
            P.barrier()

            P.mark('A2a')
            with ExitStack() as Lt:
                macc = sb(Lt, "macc", [128, 4, 512], F32)
                gate = [sb(Lt, "gate%d" % i, [128, 512], F32) for i in range(2)]
                gtmp = sb(Lt, "gtmp", [128, 512], F32)
                mergedT = sb(Lt, "mergedT", [128, DC, 512], BF16)
                brs = [(ns.w_br_pool, 4, ypoolT, 'ypoolT'), (ns.w_br_mla, 8, ymlaT, 'ymlaT'), (ns.w_br_mem, 4, ymemT, 'ymemT')]
                cntr = 0
                for half in range(2):
                    for br, (wsrc, kc, yT, yk) in enumerate(brs):
                        wb, wbk = wload(wsrc[:, half * 512:(half + 1) * 512], kc, 512)
                        wg, wgk = wload(ns.w_gate_in[:, br * 1024 + half * 512:br * 1024 + (half + 1) * 512], DC, 512)
                        for o in range(4):
                            oc = half * 4 + o
                            zb, zk = pm[cntr % 2], pmk[cntr % 2]
                            gl, glk = pm[2 + cntr % 2], pmk[2 + cntr % 2]
                            gt, gtk = gate[cntr % 2], ('gate', cntr % 2)
                            cntr += 1
                            for c in range(DC):
                                mm(gl[:, :], wg[:, c, o * 128:(o + 1) * 128], hT[:, c, 16:528], c == 0, c == DC - 1, [wgk, 'hT'], [glk])
                            for c in range(kc):
                                mm(zb[:, :], wb[:, c, o * 128:(o + 1) * 128], yT[:, c, :], c == 0, c == kc - 1, [wbk, yk], [zk])
                            bcol = br * 8 + oc
                            P.add('act', lambda e, gl=gl, gt=gt, bcol=bcol: e.activation(out=gt[:], in_=gl[:, :], func=AF.Sigmoid,
                                                                                        bias=ns.gb_t[:, bcol:bcol + 1], scale=1.0),
                                  r=[glk, 'gb_t'], w=[gtk])
                            if br == 0:
                                P.add('dve', lambda e, zb=zb, gt=gt, o=o: e.tensor_tensor(out=macc[:, o, :], in0=zb[:, :], in1=gt[:], op=ALU.mult),
                                      r=[zk, gtk], w=[('macc', o)])
                            else:
                                P.add('dve', lambda e, zb=zb, gt=gt: e.tensor_tensor(out=gtmp[:], in0=zb[:, :], in1=gt[:], op=ALU.mult),
                                      r=[zk, gtk], w=['gtmp'])
                                if br == 1:
                                    P.add('dve', lambda e, o=o: e.tensor_tensor(out=macc[:, o, :], in0=macc[:, o, :], in1=gtmp[:], op=ALU.add),
                                          r=[('macc', o), 'gtmp'], w=[('macc', o)])
                                else:
                                    P.add('dve', lambda e, o=o, oc=oc: e.tensor_tensor(out=mergedT[:, oc, :], in0=macc[:, o, :], in1=gtmp[:], op=ALU.add),
                                          r=[('macc', o), 'gtmp'], w=['mergedT'])
                wo = [wload(ns.w_out[:, hf * 512:(hf + 1) * 512], DC, 512) for hf in range(2)]
                for t in range(4):
                    s = xr_i[0] % 2
                    xr_i[0] += 1
                    xt = xr[s]
                    r0 = (G * 4 + t) * 128
                    P.add('sp', lambda e, xt=xt, r0=r0: e.dma_start(out=xt[:, :], in_=ns.x_own[r0:r0 + 128, :]), w=[('xr', s)], grp=('xr', s))
                    for hf in range(2):
                        pb, pbk = pm[4 + (2 * t + hf) % 2], pmk[4 + (2 * t + hf) % 2]
                        wv, wk = wo[hf]
                        for c in range(DC):
                            mm(pb[:, :], mergedT[:, c, t * 128:(t + 1) * 128], wv[:, c, :], c == 0, c == DC - 1, ['mergedT', wk], [pbk])
                        P.add('dve', lambda e, xt=xt, pb=pb, hf=hf: e.tensor_tensor(out=xt[:, hf * 512:(hf + 1) * 512], in0=pb[:, :],
                                                                                    in1=xt[:, hf * 512:(hf + 1) * 512], op=ALU.add),
                              r=[pbk, ('xr', s)], w=[('xr', s)])
                    P.add('sp', lambda e, xt=xt, r0=r0: e.dma_start(out=ns.x1_d[r0:r0 + 128, :], in_=xt[:, :]),
                          r=[('xr', s)], w=[('x1d', G * 4 + t)], grp=('x1st', s))
                P.barrier()


    for G_ in range(NG):
        do_group(G_)

def BUILD_B(ns):
    P, nc, cfg = ns.P, ns.nc, ns.cfg
    S, NB, NG, TO, NT = cfg.S, cfg.NB, cfg.NG, cfg.TO, cfg.NT
    sb, ps, mm, rms_rstd = ns.sb, ns.ps, ns.mm, ns.rms_rstd
    ident, stat = ns.ident, ns.stat
    ld_bcast, ld_cast = ns.ld_bcast, ns.ld_cast
    BIG = 1.0e30
    P.barrier(pe=True)
    with ExitStack() as B:
        yacc = sb(B, "yacc", [128, NT, D], F32)
        h2T = sb(B, "h2T", [128, DC, TO], BF16)
        cw = sb(B, "cw", [128, NT, N_EXP], F32)
        gffn_b = ld_bcast(B, "gffn_b", ns.g_ffn, D)
        gfin_b = ld_bcast(B, "gfin_b", ns.g_fin, D)
        brt_b = ld_bcast(B, "brt_b", ns.b_rt, 36)
        w_rt_t = ld_cast(B, "w_rt_t", ns.w_rt.rearrange("(c p) n -> p c n", p=128), [128, DC, 36])
        wg = [sb(B, "wg%d" % i, [128, DC, D_EXP], BF16) for i in range(2)]
        wu = [sb(B, "wu%d" % i, [128, DC, D_EXP], BF16) for i in range(2)]
        wd = [sb(B, "wd%d" % i, [128, 2, D], BF16) for i in range(2)]
        s_act = [sb(B, "s_act%d" % i, [128, 2, 512], BF16) for i in range(2)]
        a_act = [sb(B, "a_act%d" % i, [128, 2, 512], BF16) for i in range(2)]
        ytmp = [sb(B, "ytmp%d" % i, [128, D], F32) for i in range(2)]

        def load_expert(e_):
            i = e_ % 2
            P.add('pool', lambda e: e.dma_start(out=wg[i][:], in_=ns.w_ge[e_].rearrange("(c p) n -> p c n", p=128)),
                  w=[('wg', i)], grp=('wg', i))
            P.add('pool', lambda e: e.dma_start(out=wu[i][:], in_=ns.w_ue[e_].rearrange("(c p) n -> p c n", p=128)),
                  w=[('wu', i)], grp=('wu', i))
            P.add('pool', lambda e: e.dma_start(out=wd[i][:], in_=ns.w_de[e_].rearrange("(c p) n -> p c n", p=128)),
                  w=[('wd', i)], grp=('wd', i))

        P.mark('A2')
        load_expert(0)
        with ExitStack() as B0:
            psT = ps(B0, "psT_b", [128, 1024], BF16)
            pR = [ps(B0, "pR%d" % i, [128, 512]) for i in range(2)]
            hn2 = [sb(B0, "hn2_%d" % i, [128, D], BF16) for i in range(2)]
            lg_all = sb(B0, "lg_all", [128, NT, 36], F32)
            em = sb(B0, "em", [128, 32], F32)
            e1 = sb(B0, "e1", [128, 32], F32)
            e2 = sb(B0, "e2", [128, 32], F32)
            ge = sb(B0, "ge", [128, 4], F32)
            oh = sb(B0, "oh", [128, 4], F32)
            top8 = sb(B0, "top8", [128, 8], F32)
            rt = sb(B0, "rt", [128, 16], F32)
            def b0_a(t):
                yk = ('yacc', t)
                P.add('sp', lambda e: e.dma_start(out=yacc[:, t, :], in_=ns.x1_d[t * 128:(t + 1) * 128, :]),
                      r=[('x1d', t)], w=[yk], grp=('yl', t))
                rstd, k = rms_rstd(yacc[:, t, :], 128, D, [yk])
                hb, hbk = hn2[t % 2], ('hn2', t % 2)
                P.add('dve', lambda e: e.scalar_tensor_tensor(out=hb[:], in0=yacc[:, t, :], scalar=rstd,
                                                               in1=gffn_b[:], op0=ALU.mult, op1=ALU.mult),
                      r=[yk, k, 'gffn_b'], w=[hbk])

            def b0_b(t):
                hb, hbk = hn2[t % 2], ('hn2', t % 2)
                for c in range(DC):
                    P.add('pe', lambda e, c=c: e.transpose(out=psT[:, c * 128:(c + 1) * 128], in_=hb[:, c * 128:(c + 1) * 128],
                                                           identity=ident[:]), r=[hbk, 'ident'], w=['psTb'])
                P.add('act', lambda e: e.activation(out=h2T[:, :, t * 128:(t + 1) * 128],
                                                    in_=psT[:, :].rearrange("p (c t) -> p c t", c=DC), func=AF.Identity), r=['psTb'], w=[('h2T', t)])
                pr, prk = pR[t % 2], 'pR%d' % (t % 2)
                for c in range(DC):
                    mm(pr[:, 0:36], h2T[:, c, t * 128:(t + 1) * 128], w_rt_t[:, c, :], c == 0, c == DC - 1, [('h2T', t), 'w_rt_t'], [prk])
                P.add('dve', lambda e: e.tensor_tensor(out=lg_all[:, t, :], in0=pr[:, 0:36], in1=brt_b[:], op=ALU.add),
                      r=[prk, 'brt_b'], w=[('lg', t)])

            b0_a(0)
            for t in range(NT):
                if t + 1 < NT:
                    b0_a(t + 1)
                b0_b(t)
            LG = [('lg', t) for t in range(NT)]
            X = mybir.AxisListType.X
            gl = lg_all[:, :, 0:4]
            el4 = lg_all[:, :, 4:36].rearrange("p t (g j) -> p t g j", g=4)
            r16 = {nm: sb(B0, "r_" + nm, [128, NT], F32) for nm in ("gm", "gsum", "pg", "m1", "m2", "d", "r", "den", "w1", "w2")}
            gsh = sb(B0, "gsh", [128, NT, 4], F32)
            gex = sb(B0, "gex", [128, NT, 4], F32)
            pen = sb(B0, "pen", [128, NT, 4], F32)
            emA = sb(B0, "emA", [128, NT, 32], F32)
            emB = sb(B0, "emB", [128, NT, 32], F32)
            eq1 = sb(B0, "eq1", [128, NT, 32], F32)
            eq2 = sb(B0, "eq2", [128, NT, 32], F32)

            def bc(ap2, n):
                return ap2.unsqueeze(2).to_broadcast([128, NT, n])

            P.add('dve', lambda e: e.tensor_reduce(out=r16["gm"][:], in_=gl, axis=X, op=ALU.max), r=LG, w=['gm'])
            P.add('dve', lambda e: e.tensor_tensor(out=gsh[:], in0=gl, in1=bc(r16["gm"][:, :], 4), op=ALU.subtract), r=LG + ['gm'], w=['gsh'])
            P.add('act', lambda e: e.activation(out=gex[:], in_=gsh[:], func=AF.Exp), r=['gsh'], w=['gex'])
            P.add('dve', lambda e: e.tensor_reduce(out=r16["gsum"][:], in_=gex[:], axis=X, op=ALU.add), r=['gex'], w=['gsum'])
            P.add('dve', lambda e: e.reciprocal(out=r16["pg"][:], in_=r16["gsum"][:]), r=['gsum'], w=['pg'])
            P.add('dve', lambda e: e.tensor_scalar(out=pen[:], in0=gsh[:], scalar1=0.0, scalar2=None, op0=ALU.is_ge), r=['gsh'], w=['pen'])
            P.add('dve', lambda e: e.tensor_scalar(out=pen[:], in0=pen[:], scalar1=-1.0, scalar2=BIG, op0=ALU.add, op1=ALU.mult), r=['pen'], w=['pen'])
            P.add('dve', lambda e: e.tensor_tensor(out=emA[:].rearrange("p t (g j) -> p t g j", g=4), in0=el4,
                                                    in1=pen[:, :, :].unsqueeze(3).to_broadcast([128, NT, 4, 8]), op=ALU.add), r=LG + ['pen'], w=['emA'])
            P.add('dve', lambda e: e.tensor_reduce(out=r16["m1"][:], in_=emA[:], axis=X, op=ALU.max), r=['emA'], w=['m1'])
            P.add('dve', lambda e: e.tensor_tensor(out=eq1[:], in0=emA[:], in1=bc(r16["m1"][:, :], 32), op=ALU.is_equal), r=['emA', 'm1'], w=['eq1'])
            P.add('dve', lambda e: e.scalar_tensor_tensor(out=emB[:], in0=eq1[:], scalar=-BIG, in1=emA[:], op0=ALU.mult, op1=ALU.add),
                  r=['eq1', 'emA'], w=['emB'])
            P.add('dve', lambda e: e.tensor_reduce(out=r16["m2"][:], in_=emB[:], axis=X, op=ALU.max), r=['emB'], w=['m2'])
            P.add('dve', lambda e: e.tensor_tensor(out=eq2[:], in0=emB[:], in1=bc(r16["m2"][:, :], 32), op=ALU.is_equal), r=['emB', 'm2'], w=['eq2'])
            P.add('dve', lambda e: e.tensor_tensor(out=r16["d"][:], in0=r16["m2"][:], in1=r16["m1"][:], op=ALU.subtract), r=['m1', 'm2'], w=['d'])
            P.add('act', lambda e: e.activation(out=r16["r"][:], in_=r16["d"][:], func=AF.Exp), r=['d'], w=['r'])
            P.add('dve', lambda e: e.tensor_scalar(out=r16["den"][:], in0=r16["r"][:], scalar1=1.0, scalar2=None, op0=ALU.add), r=['r'], w=['den'])
            P.add('dve', lambda e: e.reciprocal(out=r16["den"][:], in_=r16["den"][:]), r=['den'], w=['den'])
            P.add('dve', lambda e: e.tensor_tensor(out=r16["w1"][:], in0=r16["den"][:], in1=r16["pg"][:], op=ALU.mult), r=['den', 'pg'], w=['w1'])
            P.add('dve', lambda e: e.tensor_tensor(out=r16["w2"][:], in0=r16["w1"][:], in1=r16["r"][:], op=ALU.mult), r=['w1', 'r'], w=['w2'])
            P.add('dve', lambda e: e.tensor_tensor(out=eq1[:], in0=eq1[:], in1=bc(r16["w1"][:, :], 32), op=ALU.mult), r=['eq1', 'w1'], w=['eq1'])
            P.add('dve', lambda e: e.tensor_tensor(out=eq2[:], in0=eq2[:], in1=bc(r16["w2"][:, :], 32), op=ALU.mult), r=['eq2', 'w2'], w=['eq2'])
            P.add('dve', lambda e: e.tensor_tensor(out=cw[:], in0=eq1[:], in1=eq2[:], op=ALU.add), r=['eq1', 'eq2'], w=[('cw', t) for t in range(NT)])
            P.barrier(pe=True)
        P.mark('B0')
        if ns.dbg:
            for nm, t_, shp, dt in (("d_h2T", h2T, [128, DC, TO], BF16), ("d_cw", cw, [128, NT, N_EXP], F32), ("d_x1", yacc, [128, NT, D], F32)):
                dd = nc.dram_tensor(nm, shp, dt, kind="ExternalOutput").ap()
                P.add('sp', lambda e, dd=dd, t_=t_: e.dma_start(out=dd, in_=t_[:]),
                      r=[('h2T', t) for t in range(NT)] + [('cw', t) for t in range(NT)] + [('yacc', t) for t in range(NT)], w=['outdone'], grp='out')
            P.barrier(pe=True)
        with ExitStack() as B1:
            pG = [ps(B1, "pG%d" % i, [128, 512]) for i in range(2)]
            pU = [ps(B1, "pU%d" % i, [128, 512]) for i in range(2)]
            pY = [ps(B1, "pY%d" % i, [128, 1024]) for i in range(2)]
            steps = [(e_, G) for e_ in range(N_EXP) for G in range(NG)]
            yi = [0]

            def bufs(si):
                return s_act[si % 2], ('s_act', si % 2), a_act[si % 2], ('a_act', si % 2)

            def gu(si, fc):
                e_, G = steps[si]
                i = e_ % 2
                if G == 0 and fc == 1 and e_ + 1 < N_EXP:
                    load_expert(e_ + 1)
                ts = slice(G * 512, (G + 1) * 512)
                hk = [('h2T', G * 4 + t) for t in range(4)]
                sa, sak, aa, aak = bufs(si)
                for c in range(DC):
                    mm(pG[fc][:, :], wg[i][:, c, fc * 128:(fc + 1) * 128], h2T[:, c, ts], c == 0, c == DC - 1, [('wg', i)] + hk, ['pG%d' % fc])
                P.add('act', lambda e: e.activation(out=sa[:, fc, :], in_=pG[fc][:, :], func=AF.Silu), r=['pG%d' % fc], w=[sak + (fc,)])
                for c in range(DC):
                    mm(pU[fc][:, :], wu[i][:, c, fc * 128:(fc + 1) * 128], h2T[:, c, ts], c == 0, c == DC - 1, [('wu', i)] + hk, ['pU%d' % fc])
                P.add('dve', lambda e: e.tensor_tensor(out=aa[:, fc, :], in0=pU[fc][:, :], in1=sa[:, fc, :], op=ALU.mult),
                      r=['pU%d' % fc, sak + (fc,)], w=[aak + (fc,)])

            def down(si):
                e_, G = steps[si]
                i = e_ % 2
                sa, sak, aa, aak = bufs(si)
                for t in range(4):
                    tile = G * 4 + t
                    py, pyk = pY[yi[0] % 2], 'pY%d' % (yi[0] % 2)
                    yt, ytk = ytmp[yi[0] % 2], ('ytmp', yi[0] % 2)
                    yi[0] += 1
                    for hf in range(2):
                        for fc in range(2):
                            mm(py[:, hf * 512:(hf + 1) * 512], aa[:, fc, t * 128:(t + 1) * 128], wd[i][:, fc, hf * 512:(hf + 1) * 512],
                               fc == 0, fc == 1, [aak + (fc,), ('wd', i)], [pyk])
                    P.add('act', lambda e, tile=tile, py=py, yt=yt: e.activation(
                        out=yt[:], in_=py[:, :], func=AF.Identity, scale=cw[:, tile, e_:e_ + 1]),
                        r=[pyk, ('cw', tile)], w=[ytk])
                    P.add('pool' if t % 2 == 0 else 'dve', lambda e, tile=tile, yt=yt: e.tensor_tensor(
                        out=yacc[:, tile, :], in0=yacc[:, tile, :], in1=yt[:], op=ALU.add),
                        r=[ytk, ('yacc', tile)], w=[('yacc', tile)])

            ns_ = len(steps)
            gu(0, 0)
            gu(0, 1)
            for si in range(ns_):
                if si + 1 < ns_:
                    gu(si + 1, 0)
                down(si)
                if si + 1 < ns_:
                    gu(si + 1, 1)
            P.mark('B1')
            for t in range(NT):
                yk = ('yacc', t)
                rstd, k = rms_rstd(yacc[:, t, :], 128, D, [yk])
                P.add('dve', lambda e, t=t, rstd=rstd: e.scalar_tensor_tensor(out=yacc[:, t, :], in0=yacc[:, t, :], scalar=rstd,
                                                                             in1=gfin_b[:], op0=ALU.mult, op1=ALU.mult),
                      r=[yk, k, 'gfin_b'], w=[yk])
                P.add('sp', lambda e, t=t: e.dma_start(out=ns.out[t * 128:(t + 1) * 128, :], in_=yacc[:, t, :]), r=[yk], w=['outdone'], grp='out')


def make_in_maps(cfg, inputs):
    f32 = np.float32
    x = np.asarray(inputs["x"], f32)
    B, S, _ = x.shape
    mem = np.asarray(inputs["mem"], f32)
    pos = np.asarray(inputs["positions"], np.int32)
    L = 0
    w_in = np.asarray(inputs["w_in"], f32)[L]
    w_uq = np.asarray(inputs["w_uq"], f32)[L]
    idx_n = np.concatenate([np.arange(h * 192, h * 192 + 128) for h in range(NH)])
    idx_r = np.concatenate([np.arange(h * 192 + 128, h * 192 + 192) for h in range(NH)])
    idx_rs = np.concatenate([np.concatenate([np.arange(h * 192 + 160, h * 192 + 192), np.arange(h * 192 + 128, h * 192 + 160)]) for h in range(NH)])
    krs = np.concatenate([np.arange(1152 + 32, 1152 + 64), np.arange(1152, 1152 + 32)])
    c = np.ascontiguousarray
    invf = (1.0 / (10000.0 ** (np.arange(0, 64, 2, dtype=np.float32) / 64.0))).astype(f32)
    invf2 = np.concatenate([invf, invf]).astype(f32)
    shared = {
        "invf_col": c(invf2.reshape(64, 1)), "invf_row": c(invf2.reshape(1, 64)),
        "ident": np.eye(128, dtype=ml_dtypes.bfloat16),
        "g_mix": c(np.asarray(inputs["mix_norm_g"], f32)[L].reshape(1, D)),
        "g_mem": c(np.asarray(inputs["mem_norm_g"], f32)[L].reshape(1, D)),
        "g_ffn": c(np.asarray(inputs["ffn_norm_g"], f32)[L].reshape(1, D)),
        "g_fin": c(np.asarray(inputs["final_norm_g"], f32).reshape(1, D)),
        "g_kv": c(np.asarray(inputs["kv_norm_g"], f32)[L].reshape(1, KVR)),
        "g_q": c(np.asarray(inputs["q_norm_g"], f32)[L].reshape(3, 128).T),
        "p_scale": c(np.asarray(inputs["pool_scale"], f32)[L].reshape(4, 128).T),
        "gate_b": c(np.asarray(inputs["gate_b"], f32)[L].reshape(24, 128).T),
        "b_rt": c(np.concatenate([np.asarray(inputs["b_router_group"], f32)[L], np.asarray(inputs["b_router_expert"], f32)[L]]).reshape(1, 36)),
        "w_in_pool": c(w_in[:, 0:512]), "w_in_qd": c(w_in[:, 512:896]),
        "w_in_kvx": c(np.concatenate([w_in[:, 896:1152], w_in[:, 1152:1216], w_in[:, krs]], axis=1)),
        "w_in_xq": c(w_in[:, 1216:1728]), "w_in_gate": c(w_in[:, 1728:4800]),
        "w_uq_n": c(w_uq[:, idx_n]), "w_uq_r": c(w_uq[:, idx_r]), "w_uq_rs": c(w_uq[:, idx_rs]),
        "w_uk": c(np.asarray(inputs["w_uk"], f32)[L]), "w_uv": c(np.asarray(inputs["w_uv"], f32)[L]),
        "pool_w": c(np.asarray(inputs["pool_w"], f32)[L]), "w_mem_kv": c(np.asarray(inputs["w_mem_kv"], f32)[L]),
        "w_br_pool": c(np.asarray(inputs["w_br_pool"], f32)[L]), "w_br_mla": c(np.asarray(inputs["w_br_mla"], f32)[L]),
        "w_br_mem": c(np.asarray(inputs["w_br_mem"], f32)[L]), "w_out": c(np.asarray(inputs["w_out"], f32)[L]),
        "w_rt": c(np.concatenate([np.asarray(inputs["w_router_group"], f32)[L], np.asarray(inputs["w_router_expert"], f32)[L]], axis=1)),
        "w_gate_e": c(np.asarray(inputs["w_gate_e"], f32)[L]), "w_up_e": c(np.asarray(inputs["w_up_e"], f32)[L]),
        "w_down_e": c(np.asarray(inputs["w_down_e"], f32)[L]),
    }
    tri = np.tril(np.ones((128, 128), f32)).T
    in_maps, metas = [], []
    for core in range(2 * B):
        b, half = core // 2, core % 2
        gs = own_groups(cfg, half)
        rows = np.concatenate([np.arange(g * 512, (g + 1) * 512) for g in gs])
        halo = np.zeros((cfg.NG * 16, D), f32)
        invc = np.zeros((4, cfg.TO), f32)
        mk = np.zeros((cfg.NG * 8, 128, 512), f32)
        for l, g in enumerate(gs):
            if g > 0:
                halo[l * 16:(l + 1) * 16] = x[b, g * 512 - 16:g * 512]
            tg = np.arange(g * 512, (g + 1) * 512)
            for wi, wdw in enumerate((2, 4, 8, 16)):
                invc[wi, l * 512:(l + 1) * 512] = 1.0 / np.minimum(tg + 1, wdw)
            kmax = 8 * (l + 1)
            for j in range(8):
                kb = kmax - 8 + j
                for qs in range(4):
                    qb = g * 4 + qs
                    if kb < qb:
                        mk[l * 8 + j, :, qs * 128:(qs + 1) * 128] = 1.0
                    elif kb == qb:
                        mk[l * 8 + j, :, qs * 128:(qs + 1) * 128] = tri
        m = dict(shared)
        m.update({
            "x_seq": c(x[b]), "x_own": c(x[b][rows]), "x_halo": halo,
            "pos_seq": c(pos[b].reshape(cfg.NB, 128).T), "pos_own": c(pos[b][rows].reshape(1, cfg.TO)),
            "mem": c(mem[b]), "inv_cnt": invc, "masks": mk.astype(ml_dtypes.bfloat16),
        })
        in_maps.append(m)
        metas.append((b, rows))
    return in_maps, metas


_CACHE = {}


def kernel(**inputs):
    x = np.asarray(inputs["x"])
    B, S, _ = x.shape
    cfg = Cfg(S)
    if S not in _CACHE:
        _CACHE[S] = build_program(cfg)
    nc, info = _CACHE[S]
    in_maps, metas = make_in_maps(cfg, inputs)
    res = run_bass_kernel_spmd(nc, in_maps, core_ids=list(range(2 * B)))
    outp = np.zeros((B, S, D), np.float32)
    for core, (b, rows) in enumerate(metas):
        outp[b, rows] = np.asarray(res.results[core]["out"], np.float32)
    return outp
```

```python
import math
from contextlib import ExitStack
from types import SimpleNamespace

import numpy as np
import ml_dtypes

import concourse.bass as bass
import concourse.mybir as mybir
from concourse.bass_utils import run_bass_kernel_spmd

F32 = mybir.dt.float32
BF16 = mybir.dt.bfloat16
I32 = mybir.dt.int32
AF = mybir.ActivationFunctionType
ALU = mybir.AluOpType

D = 1024
DC = 8
MEM_LEN = 256
N_EXP = 32
D_EXP = 256
QLR = 384
KVR = 256
NH = 8
XH = 4
EPS = 1e-6
PI = math.pi
TWO_PI = 2.0 * math.pi


class Prog:
    def __init__(self):
        self.ops = []
        self.barriers = []
        self.enabled = True
        self.stop = None
        self.nobar = set()
        self.excl = set(['psT', 'psTb'] + ['pm%d' % i for i in range(7)] + ['pR0', 'pR1', 'pG0', 'pG1', 'pU0', 'pU1', 'pY0', 'pY1'])

    def mark(self, name):
        if self.stop == name:
            self.enabled = False

    def add(self, eng, fn, r=(), w=(), grp=None, nobar=False):
        if not self.enabled:
            return -1
        w = tuple(w) + tuple(k for k in r if k in self.excl and k not in w)
        self.ops.append((eng, fn, tuple(r), tuple(w), grp))
        if nobar:
            self.nobar.add(len(self.ops) - 1)
        return len(self.ops) - 1

    def barrier(self, pe=False):
        self.barriers.append((len(self.ops), pe))

    def emit(self, nc, stack):
        ops = self.ops
        n = len(ops)
        last_w, readers = {}, {}
        last_on = {}
        last_grp = {}
        bar_snap = {}
        bar_snap_pe = {}
        nbar_pe = 0
        eng_bar_seen = {}
        nbar = 0
        bidx = 0
        bars = self.barriers
        need = [None] * n
        signal = [False] * n
        grp_count = {}
        grp_before = [None] * n
        for i, (eng, fn, r, w, grp) in enumerate(ops):
            while bidx < len(bars) and bars[bidx][0] <= i:
                bar_snap = dict(last_on)
                bar_snap.update({('g', g_): j_ for g_, j_ in last_grp.items()})
                nbar += 1
                if bars[bidx][1]:
                    bar_snap_pe = bar_snap
                    nbar_pe = nbar
                bidx += 1
            d = set()
            for k in r:
                j = last_w.get(k)
                if j is not None:
                    d.add(j)
            for k in w:
                j = last_w.get(k)
                if j is not None:
                    d.add(j)
                for j in readers.get(k, ()):
                    d.add(j)
            if eng == 'pe':
                if eng_bar_seen.get(eng, 0) < nbar_pe:
                    eng_bar_seen[eng] = nbar_pe
                    for j in bar_snap_pe.values():
                        d.add(j)
            elif eng_bar_seen.get(eng, 0) < nbar and i not in self.nobar:
                eng_bar_seen[eng] = nbar
                for j in bar_snap.values():
                    d.add(j)
            for k in r:
                readers.setdefault(k, []).append(i)
            for k in w:
                last_w[k] = i
                readers[k] = []
            last_on[eng] = i
            if grp is not None:
                last_grp[grp] = i
            emax, gset = {}, set()
            for j in d:
                je, _, _, _, jg = ops[j]
                if jg is not None:
                    gset.add(jg)
                else:
                    if je == 'pe' and eng == 'pe':
                        continue
                    if emax.get(je, -1) < j:
                        emax[je] = j
            for j in emax.values():
                signal[j] = True
            need[i] = (emax, {g: grp_count.get(g, 0) for g in gset})
            if grp is not None:
                grp_count[grp] = grp_count.get(grp, 0) + 1
        eng_sem = {e: stack.enter_context(nc.semaphore("sem_" + e)) for e in ('pe', 'act', 'dve', 'pool')}
        grp_sem = {g: stack.enter_context(nc.semaphore("semg_" + str(g))) for g in grp_count}
        cnt = {}
        sig_val = [0] * n
        for i, (eng, fn, r, w, grp) in enumerate(ops):
            if signal[i]:
                cnt[eng] = cnt.get(eng, 0) + 1
                sig_val[i] = cnt[eng]
        per_eng = {}
        for i, o in enumerate(ops):
            per_eng.setdefault(o[0], []).append(i)
        block = stack.enter_context(nc.Block())

        def mk(engname):
            def body(e):
                waited = {}
                for i in per_eng.get(engname, []):
                    _, fn, _, _, grp = ops[i]
                    emax, gneed = need[i]
                    for je, j in emax.items():
                        v = sig_val[j]
                        key = ('e', je)
                        if waited.get(key, 0) < v:
                            e.wait_ge(eng_sem[je], v)
                            waited[key] = v
                    for g, c in gneed.items():
                        v = 16 * c
                        key = ('g', g)
                        if v > 0 and waited.get(key, 0) < v:
                            e.wait_ge(grp_sem[g], v)
                            waited[key] = v
                    ins = fn(e)
                    if ins is None:
                        continue
                    if grp is not None:
                        ins.then_inc(grp_sem[grp], 16)
                    elif signal[i]:
                        ins.then_inc(eng_sem[engname], 1)
            return body

        block.tensor(mk('pe'))
        block.scalar(mk('act'))
        block.vector(mk('dve'))
        block.gpsimd(mk('pool'))
        block.sync(mk('sp'))
        return dict(n_ops=n, counts=cnt)


class Cfg:
    def __init__(self, S):
        self.S = S
        self.NB = S // 128
        self.NGS = S // 512
        self.NG = self.NGS // 2
        self.TO = self.NG * 512
        self.NT = self.TO // 128


def own_groups(cfg, half):
    gs = []
    for i in range(cfg.NGS // 2):
        a, b = 2 * i, 2 * i + 1
        if i % 2 == 0:
            gs.append(a if half == 0 else b)
        else:
            gs.append(b if half == 0 else a)
    return gs


def build_program(cfg, dbg=False, stop=None):
    S, NB, NG, TO, NT = cfg.S, cfg.NB, cfg.NG, cfg.TO, cfg.NT
    nc = bass.Bass("TRN2", target_bir_lowering=False)
    P = Prog()
    P.stop = stop

    def din(name, shape, dt=F32):
        return nc.dram_tensor(name, list(shape), dt, kind="ExternalInput").ap()

    x_seq = din("x_seq", [S, D]); x_own = din("x_own", [TO, D]); x_halo = din("x_halo", [NG * 16, D])
    pos_seq = din("pos_seq", [128, NB], I32); pos_own = din("pos_own", [1, TO], I32)
    mem = din("mem", [MEM_LEN, D])
    invf_col = din("invf_col", [64, 1]); invf_row = din("invf_row", [1, 64])
    inv_cnt = din("inv_cnt", [4, TO])
    masks = din("masks", [NG * 8, 128, 512], BF16)
    ident_d = din("ident", [128, 128], BF16)
    g_mix = din("g_mix", [1, D]); g_mem = din("g_mem", [1, D]); g_ffn = din("g_ffn", [1, D]); g_fin = din("g_fin", [1, D])
    g_kv = din("g_kv", [1, KVR]); g_q = din("g_q", [128, 3]); p_scale = din("p_scale", [128, 4])
    gate_b = din("gate_b", [128, 24]); b_rt = din("b_rt", [1, 36])
    w_pool_in = din("w_in_pool", [D, 512]); w_qd = din("w_in_qd", [D, QLR]); w_kvx = din("w_in_kvx", [D, 384])
    w_xq = din("w_in_xq", [D, 512]); w_gate_in = din("w_in_gate", [D, 3 * D])
    w_uq_n = din("w_uq_n", [QLR, 1024]); w_uq_r = din("w_uq_r", [QLR, 512]); w_uq_rs = din("w_uq_rs", [QLR, 512])
    w_uk = din("w_uk", [KVR, 1024]); w_uv = din("w_uv", [KVR, 1024])
    pool_w = din("pool_w", [4, 128, 128]); w_mem_kv = din("w_mem_kv", [D, 1024])
    w_br_pool = din("w_br_pool", [512, D]); w_br_mla = din("w_br_mla", [1024, D]); w_br_mem = din("w_br_mem", [512, D])
    w_out = din("w_out", [D, D]); w_rt = din("w_rt", [D, 36])
    w_ge = din("w_gate_e", [N_EXP, D, D_EXP]); w_ue = din("w_up_e", [N_EXP, D, D_EXP]); w_de = din("w_down_e", [N_EXP, D_EXP, D])
    out = nc.dram_tensor("out", [TO, D], F32, kind="ExternalOutput").ap()
    x1_d = nc.dram_tensor("x1_scr", [TO, D], F32, kind="Internal").ap()
    dbg_t = {}

    def kview(ap2d):
        return ap2d.rearrange("(c p) n -> p c n", p=128)

    uid = [0]
    with ExitStack() as top:
        def sb(stack, name, shape, dt):
            uid[0] += 1
            return stack.enter_context(nc.sbuf_tensor("s%d_%s" % (uid[0], name), list(shape), dt))

        def ps(stack, name, shape, dt=F32):
            uid[0] += 1
            return stack.enter_context(nc.psum_tensor("p%d_%s" % (uid[0], name), list(shape), dt))

        ident = sb(top, "ident", [128, 128], BF16)
        ones = sb(top, "ones", [128, 128], BF16)
        stat = sb(top, "stat", [128, 64], F32)
        junk = sb(top, "junk", [128, 1024], BF16)
        P.add('sp', lambda e: e.dma_start(out=ident[:], in_=ident_d), w=['ident'], grp='ident')
        P.add('dve', lambda e: e.memset(ones[:], 1.0), w=['ones'])
        stat_i = [0]

        def stat_slot():
            i = stat_i[0] % 16
            stat_i[0] += 1
            return i

        def ld_bcast(stack, name, src_row, n, eng='sp'):
            t = sb(stack, name, [128, n], F32)
            P.add(eng, lambda e: e.dma_start(out=t[:], in_=src_row.partition_broadcast(128)), w=[name], grp=name)
            return t

        def ld_plain(stack, name, src, shape, dt=F32):
            t = sb(stack, name, shape, dt)
            P.add('sp', lambda e: e.dma_start(out=t[:], in_=src), w=[name], grp=name)
            return t

        def ld_cast(stack, name, src_view, shape):
            t = sb(stack, name, shape, BF16)
            P.add('pool', lambda e: e.dma_start(out=t[:], in_=src_view), w=[name], grp=name)
            return t

        def mm(out_ap, lhsT, rhs, start, stop, r, w):
            P.add('pe', lambda e: e.matmul(out_ap, lhsT, rhs, start=start, stop=stop), r=r, w=w)

        def rms_rstd(src_ap, nrows, width, rkeys, psum_src=False):
            s0 = stat_slot()
            k = ('stat', s0)
            c = s0 * 4
            P.add('act', lambda e: e.activation(out=junk[:nrows, :width], in_=src_ap, func=AF.Square,
                                                scale=1.0 / math.sqrt(width), accum_out=stat[:nrows, c:c + 1]),
                  r=rkeys, w=['junk', k])
            P.add('act', lambda e: e.activation(out=stat[:nrows, c + 1:c + 2], in_=stat[:nrows, c:c + 1], func=AF.Ln,
                                                bias=EPS, scale=1.0), r=[k], w=[k])
            P.add('act', lambda e: e.activation(out=stat[:nrows, c + 2:c + 3], in_=stat[:nrows, c + 1:c + 2], func=AF.Exp,
                                                scale=-0.5), r=[k], w=[k])
            return stat[:nrows, c + 2:c + 3], k

        MAGIC = 12582912.0
        C1 = 6.28125
        C2 = TWO_PI - 6.28125

        def range_reduce(buf, tmp, bk, tk):
            P.add('dve', lambda e: e.tensor_scalar(out=tmp, in0=buf, scalar1=1.0 / TWO_PI, scalar2=MAGIC, op0=ALU.mult, op1=ALU.add),
                  r=[bk], w=[tk])
            P.add('dve', lambda e: e.tensor_scalar(out=tmp, in0=tmp, scalar1=MAGIC, scalar2=None, op0=ALU.subtract), r=[tk], w=[tk])
            P.add('dve', lambda e: e.scalar_tensor_tensor(out=buf, in0=tmp, scalar=-C1, in1=buf, op0=ALU.mult, op1=ALU.add), r=[tk, bk], w=[bk])
            P.add('dve', lambda e: e.scalar_tensor_tensor(out=buf, in0=tmp, scalar=-C2, in1=buf, op0=ALU.mult, op1=ALU.add), r=[tk, bk], w=[bk])
            P.add('dve', lambda e: e.tensor_scalar(out=buf, in0=buf, scalar1=-PI, scalar2=PI, op0=ALU.max, op1=ALU.min), r=[bk], w=[bk])

        with ExitStack() as A:
            kvT = sb(A, "kvT", [128, 3, S], BF16)
            ckvT = kvT
            ckv_tok = sb(A, "ckv_tok", [128, NB, KVR], BF16)
            gmix_b = ld_bcast(A, "gmix_b", g_mix, D)
            gkv_b = ld_bcast(A, "gkv_b", g_kv, KVR)
            invf_b = ld_bcast(A, "invf_b", invf_row, 64)
            invf_c = ld_plain(A, "invf_c", invf_col, [64, 1])
            gq_t = ld_plain(A, "gq_t", g_q, [128, 3])
            psc_t = ld_plain(A, "psc_t", p_scale, [128, 4])
            gb_t = ld_plain(A, "gb_t", gate_b, [128, 24])
            w_uv_t = ld_cast(A, "w_uv_t", kview(w_uv), [128, 2, 1024])
            poolw_t = ld_cast(A, "poolw_t", pool_w.rearrange("g c d -> c g d"), [128, 4, 128])
            w_ukT = sb(A, "w_ukT", [128, NH, KVR], BF16)
            KmemT = sb(A, "KmemT", [128, XH, MEM_LEN], BF16)
            Vmem = sb(A, "Vmem", [128, 2, 512], BF16)
            xr = [sb(A, "xr%d" % i, [128, D], F32) for i in range(2)]
            hn = [sb(A, "hn%d" % i, [128, D], BF16) for i in range(2)]
            NRING = 4
            ring = [sb(A, "ring%d" % i, [128, 4096], BF16) for i in range(NRING)]
            ring_i = [0]
            xr_i = [0]

            def wload(src2d, kc, ncols):
                s = ring_i[0] % NRING
                ring_i[0] += 1
                view = ring[s][:, 0:kc * ncols].rearrange("p (c n) -> p c n", c=kc)
                P.add('pool', lambda e: e.dma_start(out=view, in_=kview(src2d)), w=[('ring', s)], grp=('ring', s), nobar=True)
                return view, ('ring', s)

            with ExitStack() as PSA:
                psT = ps(PSA, "psT", [128, 1024], BF16)
                pm = [ps(PSA, "pm%d" % i, [128, 512]) for i in range(7)]

                def norm_tile_to_hT(src_rows, nrows, gb, gbk, hT_dst_fn):
                    s = xr_i[0] % 2
                    xr_i[0] += 1
                    xt, ht = xr[s], hn[s]
                    P.add('sp', lambda e: e.dma_start(out=xt[:nrows, :], in_=src_rows), w=[('xr', s)], grp=('xr', s), nobar=True)
                    rstd, k = rms_rstd(xt[:nrows, :], nrows, D, [('xr', s)])
                    P.add('dve', lambda e: e.scalar_tensor_tensor(out=ht[:nrows, :], in0=xt[:nrows, :], scalar=rstd,
                                                                   in1=gb[:nrows, :], op0=ALU.mult, op1=ALU.mult),
                          r=[('xr', s), k, gbk], w=[('hn', s)])
                    for c in range(DC):
                        P.add('pe', lambda e, c=c: e.transpose(out=psT[:, c * 128:c * 128 + nrows],
                                                               in_=ht[:nrows, c * 128:(c + 1) * 128],
                                                               identity=ident[:nrows, :nrows]),
                              r=[('hn', s), 'ident'], w=['psT'])
                    hT_dst_fn(psT[:, :].rearrange("p (c t) -> p c t", c=DC)[:, :, :nrows])

                def norm_a(src_rows, nrows, gb, gbk):
                    s = xr_i[0] % 2
                    xr_i[0] += 1
                    xt, ht = xr[s], hn[s]
                    P.add('sp', lambda e: e.dma_start(out=xt[:nrows, :], in_=src_rows), w=[('xr', s)], grp=('xr', s), nobar=True)
                    rstd, k = rms_rstd(xt[:nrows, :], nrows, D, [('xr', s)])
                    P.add('dve', lambda e: e.scalar_tensor_tensor(out=ht[:nrows, :], in0=xt[:nrows, :], scalar=rstd,
                                                                   in1=gb[:nrows, :], op0=ALU.mult, op1=ALU.mult),
                          r=[('xr', s), k, gbk], w=[('hn', s)])
                    return s, nrows

                def norm_b(handle, hT_dst_fn):
                    s, nrows = handle
                    ht = hn[s]
                    for c in range(DC):
                        P.add('pe', lambda e, c=c: e.transpose(out=psT[:, c * 128:c * 128 + nrows],
                                                               in_=ht[:nrows, c * 128:(c + 1) * 128],
                                                               identity=ident[:nrows, :nrows]),
                              r=[('hn', s), 'ident'], w=['psT'])
                    hT_dst_fn(psT[:, :].rearrange("p (c t) -> p c t", c=DC)[:, :, :nrows])

                P.mark('A0a')
                with ExitStack() as AU:
                    w_uk_t = ld_cast(AU, "w_uk_t", kview(w_uk), [128, 2, 1024])
                    for h in range(NH):
                        for j in range(2):
                            P.add('pe', lambda e, h=h, j=j: e.transpose(out=psT[:, j * 128:(j + 1) * 128],
                                                                        in_=w_uk_t[:, j, h * 128:(h + 1) * 128], identity=ident[:]),
                                  r=['w_uk_t', 'ident'], w=['psT'])
                        P.add('dve', lambda e, h=h: e.tensor_copy(out=w_ukT[:, h, :], in_=psT[:, 0:256]), r=['psT'], w=['w_ukT'])
                    P.barrier()
                P.mark('A0b')
                with ExitStack() as A0:
                    gmem_b = ld_bcast(A0, "gmem_b", g_mem, D)
                    memT = sb(A0, "memT", [128, DC, MEM_LEN], BF16)
                    for mt in range(2):
                        def dst(src, mt=mt):
                            P.add('dve', lambda e: e.tensor_copy(out=memT[:, :, mt * 128:(mt + 1) * 128], in_=src),
                                  r=['psT'], w=['memT'])
                        norm_tile_to_hT(mem[mt * 128:(mt + 1) * 128, :], 128, gmem_b, 'gmem_b', dst)
                    P.mark('A0c')
                    for half in range(2):
                        wv, wk = wload(w_mem_kv[:, half * 512:(half + 1) * 512], DC, 512)
                        if half == 0:
                            for hh in range(XH):
                                for c in range(DC):
                                    mm(pm[0][:, 0:MEM_LEN], wv[:, c, hh * 128:(hh + 1) * 128], memT[:, c, :], c == 0, c == DC - 1,
                                       [wk, 'memT'], ['pm0'])
                                P.add('dve', lambda e, hh=hh: e.tensor_copy(out=KmemT[:, hh, :], in_=pm[0][:, 0:MEM_LEN]),
                                      r=['pm0'], w=['KmemT'])
                        else:
                            P.mark('A0d')
                            for mt in range(2):
                                for c in range(DC):
                                    mm(pm[1][:, :], memT[:, c, mt * 128:(mt + 1) * 128], wv[:, c, :], c == 0, c == DC - 1,
                                       [wk, 'memT'], ['pm1'])
                                P.add('act', lambda e, mt=mt: e.activation(out=Vmem[:, mt, :], in_=pm[1][:, :], func=AF.Identity), r=['pm1'], w=['Vmem'])
                    P.barrier()
                P.mark('A0')

                with ExitStack() as A1:
                    w_kvx_t = ld_cast(A1, "w_kvx_t", kview(w_kvx), [128, DC, 384])
                    hTt = sb(A1, "hTt", [128, DC, 128], BF16)
                    kro = sb(A1, "kro", [128, 128], BF16)
                    P.add('dve', lambda e: e.memset(kro[:], 0.0), w=[('kro', 0)])
                    tA = sb(A1, "tA", [128, 64], F32)
                    tB = sb(A1, "tB", [128, 64], F32)
                    posi = sb(A1, "posi", [128, NB], I32)
                    posf = sb(A1, "posf", [128, NB], F32)
                    angc = sb(A1, "angc", [128, NB, 64], F32)
                    angs = sb(A1, "angs", [128, NB, 64], F32)
                    P.add('sp', lambda e: e.dma_start(out=posi[:], in_=pos_seq),
                          w=['posi'], grp='posi')
                    P.add('dve', lambda e: e.tensor_copy(out=posf[:], in_=posi[:]), r=['posi'], w=['posf'])
                    for t in range(NB):
                        P.add('dve', lambda e, t=t: e.tensor_scalar(out=angc[:, t, :], in0=invf_b[:, :], scalar1=posf[:, t:t + 1],
                                                                     scalar2=PI / 2, op0=ALU.mult, op1=ALU.add),
                              r=['posf', 'invf_b'], w=['angc'])
                        P.add('dve', lambda e, t=t: e.tensor_scalar(out=angs[:, t, :], in0=invf_b[:, :], scalar1=posf[:, t:t + 1],
                                                                     scalar2=0.0, op0=ALU.mult, op1=ALU.add),
                              r=['posf', 'invf_b'], w=['angs'])
                    angt = sb(A1, "angt", [128, NB, 64], F32)
                    range_reduce(angc[:], angt[:], 'angc', 'angt')
                    range_reduce(angs[:], angt[:], 'angs', 'angt')
                    P.add('act', lambda e: e.activation(out=angc[:], in_=angc[:], func=AF.Sin, scale=1.0),
                          r=['angc'], w=['angc'])
                    P.add('act', lambda e: e.activation(out=angs[:, :, 0:32], in_=angs[:, :, 0:32], func=AF.Sin, scale=-1.0),
                          r=['angs'], w=['angs'])
                    P.add('act', lambda e: e.activation(out=angs[:, :, 32:64], in_=angs[:, :, 32:64], func=AF.Sin, scale=1.0),
                          r=['angs'], w=['angs'])
                    P.mark('A1a')
                    hTt2 = [hTt, sb(A1, "hTt_b", [128, DC, 128], BF16)]
                    kro2 = [kro, sb(A1, "kro_b", [128, 128], BF16)]
                    P.add('dve', lambda e: e.memset(kro2[1][:], 0.0), w=[('kro', 1)])
                    tA2 = [tA, sb(A1, "tA_b", [128, 64], F32)]
                    tB2 = [tB, sb(A1, "tB_b", [128, 64], F32)]

                    psT2 = pm[6][:, :].bitcast(BF16)

                    def front_a(t):
                        s = t % 2
                        xt, ht = xr[s], hn[s]
                        P.add('sp', lambda e: e.dma_start(out=xt[:, :], in_=x_seq[t * 128:(t + 1) * 128, :]), w=[('xr', s)], grp=('xr', s),
                              nobar=True)
                        rstd, k = rms_rstd(xt[:, :], 128, D, [('xr', s)])
                        P.add('dve', lambda e: e.scalar_tensor_tensor(out=ht[:, :], in0=xt[:, :], scalar=rstd, in1=gmix_b[:, :],
                                                                       op0=ALU.mult, op1=ALU.mult),
                              r=[('xr', s), k, 'gmix_b'], w=[('hn', s)])

                    def front_b(t):
                        s = t % 2
                        ht = hn[s]
                        hb, hk = hTt2[t % 2], ('hTt', t % 2)
                        for c in range(DC):
                            P.add('pe', lambda e, c=c: e.transpose(out=psT[:, c * 128:(c + 1) * 128], in_=ht[:, c * 128:(c + 1) * 128],
                                                                   identity=ident[:]), r=[('hn', s), 'ident'], w=['psT'])
                        P.add('dve', lambda e: e.tensor_copy(out=hb[:], in_=psT[:, :].rearrange("p (c t) -> p c t", c=DC)), r=['psT'], w=[hk])
                        pk, pkk = pm[t % 2], 'pm%d' % (t % 2)
                        for c in range(DC):
                            mm(pk[:, 0:384], hb[:, c, :], w_kvx_t[:, c, :], c == 0, c == DC - 1, [hk, 'w_kvx_t'], [pkk])

                    def back(t):
                        pk, pkk = pm[t % 2], 'pm%d' % (t % 2)
                        ta, tb, kr = tA2[t % 2], tB2[t % 2], kro2[t % 2]
                        tak, tbk, krk = ('tA', t % 2), ('tB', t % 2), ('kro', t % 2)
                        rstd, k = rms_rstd(pk[:, 0:KVR], 128, KVR, [pkk])
                        P.add('dve', lambda e: e.scalar_tensor_tensor(
                            out=ckv_tok[:, t, :], in0=pk[:, 0:KVR], scalar=rstd, in1=gkv_b[:, :], op0=ALU.mult, op1=ALU.mult),
                            r=[pkk, k, 'gkv_b'], w=[('ckv_tok', t)])
                        P.add('dve', lambda e: e.tensor_tensor(out=ta[:], in0=pk[:, 256:320], in1=angc[:, t, :], op=ALU.mult),
                              r=[pkk, 'angc'], w=[tak])
                        P.add('dve', lambda e: e.tensor_tensor(out=tb[:], in0=pk[:, 320:384], in1=angs[:, t, :], op=ALU.mult),
                              r=[pkk, 'angs'], w=[tbk])
                        P.add('dve', lambda e: e.tensor_tensor(out=kr[:, 0:64], in0=ta[:], in1=tb[:], op=ALU.add), r=[tak, tbk], w=[krk])
                        for j in range(2):
                            P.add('pe', lambda e, j=j: e.transpose(out=psT2[:, j * 128:(j + 1) * 128],
                                                                   in_=ckv_tok[:, t, j * 128:(j + 1) * 128], identity=ident[:]),
                                  r=[('ckv_tok', t), 'ident'], w=['pm6'])
                        P.add('pe', lambda e: e.transpose(out=psT2[:, 256:384], in_=kr[:, :], identity=ident[:]),
                              r=[krk, 'ident'], w=['pm6'])
                        P.add('dve', lambda e: e.tensor_copy(out=kvT[:, :, t * 128:(t + 1) * 128],
                                                             in_=psT2[:, 0:384].rearrange("p (j t) -> p j t", j=3)),
                              r=['pm6'], w=[('ckvT', t), ('kropeT', t)])

                    front_a(0)
                    front_a(1)
                    front_b(0)
                    for t in range(NB):
                        if t + 1 < NB:
                            front_b(t + 1)
                        if t + 2 < NB:
                            front_a(t + 2)
                        back(t)
                    xr_i[0] = 0
                    P.barrier()
                P.mark('A1')
                if dbg:
                    for nm, t_, shp, dt in (("d_ckvT", ckvT, [128, 3, S], BF16),
                                            ("d_KmemT", KmemT, [128, XH, MEM_LEN], BF16), ("d_Vmem", Vmem, [128, 2, 512], BF16)):
                        dd = nc.dram_tensor(nm, shp, dt, kind="ExternalOutput").ap()
                        P.add('sp', lambda e, dd=dd, t_=t_: e.dma_start(out=dd, in_=t_[:]),
                              r=[('ckvT', t) for t in range(NB)] + [('kropeT', t) for t in range(NB)] + ['KmemT', 'Vmem'],
                              w=['outdone'], grp='out')
                BUILD_A2(SimpleNamespace(**locals()))
        BUILD_B(SimpleNamespace(**locals()))
        P.add('sp', lambda e: None, r=['outdone'])
        info = P.emit(nc, top)
    return nc, info


def BUILD_A2(ns):
    P, nc, cfg = ns.P, ns.nc, ns.cfg
    S, NB, NG, TO, NT = cfg.S, cfg.NB, cfg.NG, cfg.TO, cfg.NT
    sb, mm, wload, rms_rstd, norm_tile_to_hT = ns.sb, ns.mm, ns.wload, ns.rms_rstd, ns.norm_tile_to_hT
    pm, psT, ident, ones = ns.pm, ns.psT, ns.ident, ns.ones
    ckvT, ckv_tok = ns.ckvT, ns.ckv_tok
    xr, xr_i = ns.xr, ns.xr_i
    SC_MLA = 1.0 / math.sqrt(192.0)
    SC_MEM = 1.0 / math.sqrt(128.0)
    pmk = ['pm%d' % i for i in range(7)]
    rot = [0]

    def bank():
        b = rot[0] % 7
        rot[0] += 1
        return pm[b], pmk[b]

    def do_group(G):
        kmax = 8 * (G + 1)
        with ExitStack() as GS:
            hT = sb(GS, "hT", [128, DC, 528], BF16)
            xqT = sb(GS, "xqT", [128, XH, 512], BF16)
            cqT = sb(GS, "cqT", [128, 3, 512], BF16)
            cos2 = sb(GS, "cos2", [64, 512], F32)
            sin2s = sb(GS, "sin2s", [64, 512], F32)
            qabs = sb(GS, "qabs", [128, NH, 2, 512], BF16)
            qr = sb(GS, "qr", [128, NH, 512], BF16)
            P.add('dve', lambda e: e.memset(qr[64:128, :, :], 0.0), w=[('qr', h_) for h_ in range(NH)])
            PT = [sb(GS, "PT%d" % i, [128, 512], BF16) for i in range(3)]
            mk = [sb(GS, "mk%d" % i, [128, 512], BF16) for i in range(2)]
            rs = sb(GS, "rs", [128, 512], F32)
            olat = sb(GS, "olat", [128, 2, 512], BF16)
            ocp = sb(GS, "ocp", [128, 2, 512], F32)
            ymlaT = sb(GS, "ymlaT", [128, NH, 512], BF16)
            ymemT = sb(GS, "ymemT", [128, XH, 512], BF16)
            ypoolT = sb(GS, "ypoolT", [128, 4, 512], BF16)
            PTm = sb(GS, "PTm", [128, 2, 512], BF16)

            def dst_h(src):
                P.add('act', lambda e: e.activation(out=hT[:, :, 0:16], in_=src, func=AF.Identity), r=['psT'], w=['hT'])

            def mk_dst(t):
                def dst_t(src):
                    P.add('dve', lambda e: e.tensor_copy(out=hT[:, :, 16 + t * 128:16 + (t + 1) * 128], in_=src),
                          r=['psT'], w=['hT'])
                return dst_t
            jobs = [(ns.x_halo[G * 16:(G + 1) * 16, :], 16, dst_h)]
            for t in range(4):
                r0 = (G * 4 + t) * 128
                jobs.append((ns.x_own[r0:r0 + 128, :], 128, mk_dst(t)))
            hs = {}
            hs[0] = ns.norm_a(jobs[0][0], jobs[0][1], ns.gmix_b, 'gmix_b')
            hs[1] = ns.norm_a(jobs[1][0], jobs[1][1], ns.gmix_b, 'gmix_b')
            for i_ in range(5):
                ns.norm_b(hs[i_], jobs[i_][2])
                if i_ + 2 < 5:
                    hs[i_ + 2] = ns.norm_a(jobs[i_ + 2][0], jobs[i_ + 2][1], ns.gmix_b, 'gmix_b')

            with ExitStack() as E:
                u = [sb(E, "u%d" % i, [128, 528], F32) for i in range(2)]
                sAB = [sb(E, "sA", [128, 528], F32), sb(E, "sB", [128, 528], F32)]
                invc = [sb(E, "invc%d" % i, [128, 512], F32) for i in range(2)]
                tmpp = sb(E, "tmpp", [128, 512], F32)
                pT = [sb(E, "pT%d" % i, [128, 512], BF16) for i in range(2)]
                qdT = sb(E, "qdT", [128, 3, 512], F32)
                sq = sb(E, "sq", [128, 3, 512], BF16)
                lnq = sb(E, "lnq", [128, 512], F32)
                rstdq = lnq
                wv, wk = wload(ns.w_pool_in, DC, 512)
                for g in range(4):
                    ug, uk = u[g % 2], ('u', g % 2)
                    pb, pbk = bank()
                    for c in range(DC):
                        mm(pb[:, :], wv[:, c, g * 128:(g + 1) * 128], hT[:, c, 16:528], c == 0, c == DC - 1, [wk, 'hT'], [pbk])
                    P.add('act', lambda e, ug=ug, pb=pb: e.activation(out=ug[:, 16:528], in_=pb[:, :], func=AF.Identity), r=[pbk], w=[uk])
                    ph, phk = bank()
                    for c in range(DC):
                        mm(ph[:, 0:16], wv[:, c, g * 128:(g + 1) * 128], hT[:, c, 0:16], c == 0, c == DC - 1, [wk, 'hT'], [phk])
                    P.add('dve', lambda e, ug=ug, ph=ph: e.tensor_copy(out=ug[:, 0:16], in_=ph[:, 0:16]), r=[phk], w=[uk])
                    src, srck = ug, uk
                    for k in range(g + 1):
                        sh = 1 << k
                        lo = 2 * sh - 1
                        dstb, dstk = sAB[k % 2], ('sAB', k % 2)
                        P.add('dve', lambda e, dstb=dstb, src=src, lo=lo, sh=sh: e.tensor_tensor(
                            out=dstb[:, lo:528], in0=src[:, lo:528], in1=src[:, lo - sh:528 - sh], op=ALU.add),
                            r=[srck], w=[dstk])
                        src, srck = dstb, dstk
                    ic, ick = invc[g % 2], ('invc', g % 2)
                    P.add('sp', lambda e, ic=ic, g=g: e.dma_start(
                        out=ic[:], in_=ns.inv_cnt[g:g + 1, G * 512:(G + 1) * 512].partition_broadcast(128)), w=[ick], grp=ick)
                    P.add('dve', lambda e, src=src, ic=ic: e.tensor_tensor(out=tmpp[:], in0=src[:, 16:528], in1=ic[:], op=ALU.mult),
                          r=[srck, ick], w=['tmpp'])
                    pTg, pTk = pT[g % 2], ('pT', g % 2)
                    P.add('dve', lambda e, pTg=pTg, ug=ug: e.tensor_tensor(out=pTg[:], in0=tmpp[:], in1=ug[:, 16:528], op=ALU.subtract),
                          r=['tmpp', uk], w=[pTk])
                    py, pyk = bank()
                    mm(py[:, :], ns.poolw_t[:, g, :], pTg[:, :], True, True, ['poolw_t', pTk], [pyk])
                    P.add('dve', lambda e, g=g, py=py: e.tensor_scalar(out=ypoolT[:, g, :], in0=py[:, :], scalar1=ns.psc_t[:, g:g + 1],
                                                                        scalar2=None, op0=ALU.mult), r=[pyk, 'psc_t'], w=['ypoolT'])
                wv, wk = wload(ns.w_qd, DC, QLR)
                for oc in range(3):
                    pb, pbk = bank()
                    for c in range(DC):
                        mm(pb[:, :], wv[:, c, oc * 128:(oc + 1) * 128], hT[:, c, 16:528], c == 0, c == DC - 1, [wk, 'hT'], [pbk])
                    P.add('dve', lambda e, oc=oc, pb=pb: e.tensor_copy(out=qdT[:, oc, :], in_=pb[:, :]), r=[pbk], w=[('qdT', oc)])
                    P.add('act', lambda e, oc=oc, pb=pb: e.activation(out=sq[:, oc, :], in_=pb[:, :], func=AF.Square), r=[pbk], w=[('sq', oc)])
                pz, pzk = bank()
                for oc in range(3):
                    mm(pz[:, :], ones[:, :], sq[:, oc, :], oc == 0, oc == 2, ['ones', ('sq', oc)], [pzk])
                P.add('act', lambda e, pz=pz: e.activation(out=lnq[:], in_=pz[:, :], func=AF.Ln, bias=EPS, scale=1.0 / QLR), r=[pzk], w=['lnq'])
                P.add('act', lambda e: e.activation(out=lnq[:], in_=lnq[:], func=AF.Exp, scale=-0.5), r=['lnq'], w=['lnq', 'rstdq'])
                for oc in range(3):
                    P.add('dve', lambda e, oc=oc: e.scalar_tensor_tensor(out=cqT[:, oc, :], in0=qdT[:, oc, :], scalar=ns.gq_t[:, oc:oc + 1],
                                                                         in1=rstdq[:], op0=ALU.mult, op1=ALU.mult),
                          r=[('qdT', oc), 'gq_t', 'rstdq'], w=['cqT'])
                wv, wk = wload(ns.w_xq, DC, 512)
                for oc in range(XH):
                    pb, pbk = bank()
                    for c in range(DC):
                        mm(pb[:, :], wv[:, c, oc * 128:(oc + 1) * 128], hT[:, c, 16:528], c == 0, c == DC - 1, [wk, 'hT'], [pbk])
                    P.add('act', lambda e, oc=oc, pb=pb: e.activation(out=xqT[:, oc, :], in_=pb[:, :], func=AF.Identity), r=[pbk], w=['xqT'])
                P.barrier()
            P.mark('A2e')

            with ExitStack() as M:
                qn = sb(M, "qn", [128, NH, 512], BF16)
                t1 = sb(M, "t1", [64, 512], F32)
                t2 = sb(M, "t2", [64, 512], F32)
                posb = sb(M, "posb", [64, 512], I32)
                P.add('sp', lambda e: e.dma_start(out=posb[:], in_=ns.pos_own[0:1, G * 512:(G + 1) * 512].partition_broadcast(64)),
                      w=['posb'], grp='posb')
                P.add('dve', lambda e: e.tensor_copy(out=t1[:], in_=posb[:]), r=['posb'], w=['t1'])
                P.add('dve', lambda e: e.tensor_scalar(out=cos2[:], in0=t1[:], scalar1=ns.invf_c[:, 0:1], scalar2=PI / 2,
                                                        op0=ALU.mult, op1=ALU.add), r=['t1', 'invf_c'], w=['cos2'])
                P.add('dve', lambda e: e.tensor_scalar(out=sin2s[:], in0=t1[:], scalar1=ns.invf_c[:, 0:1], scalar2=0.0,
                                                        op0=ALU.mult, op1=ALU.add), r=['t1', 'invf_c'], w=['sin2s'])
                ns.range_reduce(cos2[:], t2[:], 'cos2', 't2')
                ns.range_reduce(sin2s[:], t2[:], 'sin2s', 't2')
                P.add('act', lambda e: e.activation(out=cos2[:], in_=cos2[:], func=AF.Sin, scale=1.0), r=['cos2'], w=['cos2'])
                P.add('act', lambda e: e.activation(out=sin2s[0:32, :], in_=sin2s[0:32, :], func=AF.Sin, scale=-1.0),
                      r=['sin2s'], w=['sin2s'])
                P.add('act', lambda e: e.activation(out=sin2s[32:64, :], in_=sin2s[32:64, :], func=AF.Sin, scale=1.0),
                      r=['sin2s'], w=['sin2s'])
                for hh in range(XH):
                    sbk = []
                    for mb in range(2):
                        pb, pbk = bank()
                        mm(pb[:, :], ns.KmemT[:, hh, mb * 128:(mb + 1) * 128], xqT[:, hh, :], True, True, ['KmemT', 'xqT'], [pbk])
                        P.add('act', lambda e, mb=mb, pb=pb: e.activation(out=PTm[:, mb, :], in_=pb[:, :], func=AF.Exp, scale=SC_MEM),
                              r=[pbk], w=[('PTm', mb)])
                    po, pok = bank()
                    pz, pzk = bank()
                    for mb in range(2):
                        mm(po[:, :], ns.Vmem[:, mb, hh * 128:(hh + 1) * 128], PTm[:, mb, :], mb == 0, mb == 1, ['Vmem', ('PTm', mb)], [pok])
                    for mb in range(2):
                        mm(pz[:, :], ones[:, :], PTm[:, mb, :], mb == 0, mb == 1, ['ones', ('PTm', mb)], [pzk])
                    P.add('dve', lambda e, pz=pz: e.reciprocal(out=rs[:], in_=pz[:, :]), r=[pzk], w=['rs'])
                    P.add('dve', lambda e, hh=hh, po=po: e.tensor_tensor(out=ymemT[:, hh, :], in0=po[:, :], in1=rs[:], op=ALU.mult),
                          r=[pok, 'rs'], w=['ymemT'])
                wn, wnk = wload(ns.w_uq_n, 3, 1024)
                wr, wrk = wload(ns.w_uq_r, 3, 512)
                wrs, wrsk = wload(ns.w_uq_rs, 3, 512)
                for h in range(NH):
                    pb, pbk = bank()
                    for c in range(3):
                        mm(pb[:, :], wn[:, c, h * 128:(h + 1) * 128], cqT[:, c, :], c == 0, c == 2, [wnk, 'cqT'], [pbk])
                    P.add('act', lambda e, h=h, pb=pb: e.activation(out=qn[:, h, :], in_=pb[:, :], func=AF.Identity), r=[pbk], w=[('qn', h)])
                for h in range(NH):
                    for j in range(2):
                        pb, pbk = bank()
                        mm(pb[:, :], ns.w_ukT[:, h, j * 128:(j + 1) * 128], qn[:, h, :], True, True, ['w_ukT', ('qn', h)], [pbk])
                        if j == 0:
                            P.add('act', lambda e, h=h, j=j, pb=pb: e.activation(out=qabs[:, h, j, :], in_=pb[:, :], func=AF.Identity), r=[pbk], w=[('qabs', h)])
                        else:
                            P.add('dve', lambda e, h=h, j=j, pb=pb: e.tensor_copy(out=qabs[:, h, j, :], in_=pb[:, :]), r=[pbk], w=[('qabs', h)])
                for h in range(NH):
                    pa, pak = bank()
                    for c in range(3):
                        mm(pa[0:64, :], wr[:, c, h * 64:(h + 1) * 64], cqT[:, c, :], c == 0, c == 2, [wrk, 'cqT'], [pak])
                    P.add('dve', lambda e, pa=pa: e.tensor_tensor(out=t1[:], in0=pa[0:64, :], in1=cos2[:], op=ALU.mult), r=[pak, 'cos2'], w=['t1'])
                    pb, pbk = bank()
                    for c in range(3):
                        mm(pb[0:64, :], wrs[:, c, h * 64:(h + 1) * 64], cqT[:, c, :], c == 0, c == 2, [wrsk, 'cqT'], [pbk])
                    P.add('dve', lambda e, pb=pb: e.tensor_tensor(out=t2[:], in0=pb[0:64, :], in1=sin2s[:], op=ALU.mult), r=[pbk, 'sin2s'], w=['t2'])
                    P.add('dve', lambda e, h=h: e.tensor_tensor(out=qr[0:64, h, :], in0=t1[:], in1=t2[:], op=ALU.add), r=['t1', 't2'], w=[('qr', h)])
                P.barrier()

            P.mark('A2m')
            cells = [(h, kb) for h in range(NH) for kb in range(kmax)]
            SB = [(pm[0], 'pm0'), (pm[1], 'pm1'), (pm[2], 'pm2')]
            O0, O1, Z = pm[3], pm[4], pm[5]
            deferred = []
            mki = [0]

            def emit_qk(i):
                h, kb = cells[i]
                sbk_, sk = SB[i % 3]
                ks = slice(kb * 128, (kb + 1) * 128)
                mm(sbk_[:, :], ckvT[:, 0, ks], qabs[:, h, 0, :], True, False, [('ckvT', kb), ('qabs', h)], [sk])
                mm(sbk_[:, :], ckvT[:, 1, ks], qabs[:, h, 1, :], False, False, [('ckvT', kb), ('qabs', h)], [sk])
                mm(sbk_[:, :], ckvT[:, 2, ks], qr[:, h, :], False, True, [('kropeT', kb), ('qr', h)], [sk])

            def emit_rest(i):
                h, kb = cells[i]
                sbk_, sk = SB[i % 3]
                pt, ptk = PT[i % 3], ('PT', i % 3)
                P.add('act', lambda e: e.activation(out=pt[:], in_=sbk_[:, :], func=AF.Exp, scale=SC_MLA), r=[sk], w=[ptk])
                if kb >= kmax - 8:
                    m, mkk = mk[mki[0] % 2], ('mk', mki[0] % 2)
                    mki[0] += 1
                    mi = G * 8 + (kb - (kmax - 8))
                    P.add('sp', lambda e: e.dma_start(out=m[:], in_=ns.masks[mi]), w=[mkk], grp=mkk)
                    P.add('dve', lambda e: e.tensor_tensor(out=pt[:], in0=pt[:], in1=m[:], op=ALU.mult), r=[ptk, mkk], w=[ptk])
                first, last = kb == 0, kb == kmax - 1
                mm(O0[:, :], ckv_tok[:, kb, 0:128], pt[:], first, last, [('ckv_tok', kb), ptk], ['pm3'])
                mm(O1[:, :], ckv_tok[:, kb, 128:256], pt[:], first, last, [('ckv_tok', kb), ptk], ['pm4'])
                mm(Z[:, :], ones[:, :], pt[:], first, last, ['ones', ptk], ['pm5'])
                if last:
                    P.add('act', lambda e: e.activation(out=ocp[:, 0, :], in_=O0[:, :], func=AF.Identity), r=['pm3'], w=[('ocp', 0)])
                    P.add('dve', lambda e: e.tensor_copy(out=ocp[:, 1, :], in_=O1[:, :]), r=['pm4'], w=[('ocp', 1)])
                    P.add('dve', lambda e: e.reciprocal(out=rs[:], in_=Z[:, :]), r=['pm5'], w=['rs'])
                    P.add('dve', lambda e: e.tensor_tensor(out=olat[:, 0, :], in0=ocp[:, 0, :], in1=rs[:], op=ALU.mult), r=[('ocp', 0), 'rs'], w=['olat'])
                    P.add('dve', lambda e: e.tensor_tensor(out=olat[:, 1, :], in0=ocp[:, 1, :], in1=rs[:], op=ALU.mult), r=[('ocp', 1), 'rs'], w=['olat'])
                    deferred.append((h, i + 3))

            def flush(i, force=False):
                while deferred and (force or deferred[0][1] <= i):
                    h, _ = deferred.pop(0)
                    for j in range(2):
                        mm(pm[6][:, :], ns.w_uv_t[:, j, h * 128:(h + 1) * 128], olat[:, j, :], j == 0, j == 1, ['w_uv_t', 'olat'], ['pm6'])
                    P.add('act', lambda e, h=h: e.activation(out=ymlaT[:, h, :], in_=pm[6][:, :], func=AF.Identity), r=['pm6'], w=['ymlaT'])

            nc_ = len(cells)
            for i in range(nc_ + 2):
                if i < nc_:
                    emit_qk(i)
                if i >= 2:
                    emit_rest(i - 2)
                    flush(i - 2)
            flush(0, force=True)
            P.barrier()

            P.mark('A2a')
            with ExitStack() as Lt:
                macc = sb(Lt, "macc", [128, 4, 512], F32)
                gate = [sb(Lt, "gate%d" % i, [128, 512], F32) for i in range(2)]
                gtmp = sb(Lt, "gtmp", [128, 512], F32)
                mergedT = sb(Lt, "mergedT", [128, DC, 512], BF16)
                brs = [(ns.w_br_pool, 4, ypoolT, 'ypoolT'), (ns.w_br_mla, 8, ymlaT, 'ymlaT'), (ns.w_br_mem, 4, ymemT, 'ymemT')]
                cntr = 0
                for half in range(2):
                    for br, (wsrc, kc, yT, yk) in enumerate(brs):
                        wb, wbk = wload(wsrc[:, half * 512:(half + 1) * 512], kc, 512)
                        wg, wgk = wload(ns.w_gate_in[:, br * 1024 + half * 512:br * 1024 + (half + 1) * 512], DC, 512)
                        for o in range(4):
                            oc = half * 4 + o
                            zb, zk = pm[cntr % 2], pmk[cntr % 2]
                            gl, glk = pm[2 + cntr % 2], pmk[2 + cntr % 2]
                            gt, gtk = gate[cntr % 2], ('gate', cntr % 2)
                            cntr += 1
                            for c in range(DC):
                                mm(gl[:, :], wg[:, c, o * 128:(o + 1) * 128], hT[:, c, 16:528], c == 0, c == DC - 1, [wgk, 'hT'], [glk])
                            for c in range(kc):
                                mm(zb[:, :], wb[:, c, o * 128:(o + 1) * 128], yT[:, c, :], c == 0, c == kc - 1, [wbk, yk], [zk])
                            bcol = br * 8 + oc
                            P.add('act', lambda e, gl=gl, gt=gt, bcol=bcol: e.activation(out=gt[:], in_=gl[:, :], func=AF.Sigmoid,
                                                                                        bias=ns.gb_t[:, bcol:bcol + 1], scale=1.0),
                                  r=[glk, 'gb_t'], w=[gtk])
                            if br == 0:
                                P.add('dve', lambda e, zb=zb, gt=gt, o=o: e.tensor_tensor(out=macc[:, o, :], in0=zb[:, :], in1=gt[:], op=ALU.mult),
                                      r=[zk, gtk], w=[('macc', o)])
                            else:
                                P.add('dve', lambda e, zb=zb, gt=gt: e.tensor_tensor(out=gtmp[:], in0=zb[:, :], in1=gt[:], op=ALU.mult),
                                      r=[zk, gtk], w=['gtmp'])
                                if br == 1:
                                    P.add('dve', lambda e, o=o: e.tensor_tensor(out=macc[:, o, :], in0=macc[:, o, :], in1=gtmp[:], op=ALU.add),
                                          r=[('macc', o), 'gtmp'], w=[('macc', o)])
                                else:
                                    P.add('dve', lambda e, o=o, oc=oc: e.tensor_tensor(out=mergedT[:, oc, :], in0=macc[:, o, :], in1=gtmp[:], op=ALU.add),
                                          r=[('macc', o), 'gtmp'], w=['mergedT'])
                wo = [wload(ns.w_out[:, hf * 512:(hf + 1) * 512], DC, 512) for hf in range(2)]
                for t in range(4):
                    s = xr_i[0] % 2
                    xr_i[0] += 1
                    xt = xr[s]
                    r0 = (G * 4 + t) * 128
                    P.add('sp', lambda e, xt=xt, r0=r0: e.dma_start(out=xt[:, :], in_=ns.x_own[r0:r0 + 128, :]), w=[('xr', s)], grp=('xr', s))
                    for hf in range(2):
                        pb, pbk = pm[4 + (2 * t + hf) % 2], pmk[4 + (2 * t + hf) % 2]
                        wv, wk = wo[hf]
                        for c in range(DC):
                            mm(pb[:, :], mergedT[:, c, t * 128:(t + 1) * 128], wv[:, c, :], c == 0, c == DC - 1, ['mergedT', wk], [pbk])
                        P.add('dve', lambda e, xt=xt, pb=pb, hf=hf: e.tensor_tensor(out=xt[:, hf * 512:(hf + 1) * 512], in0=pb[:, :],
                                                                                    in1=xt[:, hf * 512:(hf + 1) * 512], op=ALU.add),
                              r=[pbk, ('xr', s)], w=[('xr', s)])
                    P.add('sp', lambda e, xt=xt, r0=r0: e.dma_start(out=ns.x1_d[r0:r0 + 128, :], in_=xt[:, :]),
                          r=[('xr', s)], w=[('x1d', G * 4 + t)], grp=('x1st', s))
                P.barrier()


    for G_ in range(NG):
        do_group(G_)

def BUILD_B(ns):
    P, nc, cfg = ns.P, ns.nc, ns.cfg
    S, NB, NG, TO, NT = cfg.S, cfg.NB, cfg.NG, cfg.TO, cfg.NT
    sb, ps, mm, rms_rstd = ns.sb, ns.ps, ns.mm, ns.rms_rstd
    ident, stat = ns.ident, ns.stat
    ld_bcast, ld_cast = ns.ld_bcast, ns.ld_cast
    BIG = 1.0e30
    P.barrier(pe=True)
    with ExitStack() as B:
        yacc = sb(B, "yacc", [128, NT, D], F32)
        h2T = sb(B, "h2T", [128, DC, TO], BF16)
        cw = sb(B, "cw", [128, NT, N_EXP], F32)
        gffn_b = ld_bcast(B, "gffn_b", ns.g_ffn, D)
        gfin_b = ld_bcast(B, "gfin_b", ns.g_fin, D)
        brt_b = ld_bcast(B, "brt_b", ns.b_rt, 36)
        w_rt_t = ld_cast(B, "w_rt_t", ns.w_rt.rearrange("(c p) n -> p c n", p=128), [128, DC, 36])
        wg = [sb(B, "wg%d" % i, [128, DC, D_EXP], BF16) for i in range(2)]
        wu = [sb(B, "wu%d" % i, [128, DC, D_EXP], BF16) for i in range(2)]
        wd = [sb(B, "wd%d" % i, [128, 2, D], BF16) for i in range(2)]
        s_act = [sb(B, "s_act%d" % i, [128, 2, 512], BF16) for i in range(2)]
        a_act = [sb(B, "a_act%d" % i, [128, 2, 512], BF16) for i in range(2)]
        ytmp = [sb(B, "ytmp%d" % i, [128, D], F32) for i in range(2)]

        def load_expert(e_):
            i = e_ % 2
            P.add('pool', lambda e: e.dma_start(out=wg[i][:], in_=ns.w_ge[e_].rearrange("(c p) n -> p c n", p=128)),
                  w=[('wg', i)], grp=('wg', i))
            P.add('pool', lambda e: e.dma_start(out=wu[i][:], in_=ns.w_ue[e_].rearrange("(c p) n -> p c n", p=128)),
                  w=[('wu', i)], grp=('wu', i))
            P.add('pool', lambda e: e.dma_start(out=wd[i][:], in_=ns.w_de[e_].rearrange("(c p) n -> p c n", p=128)),
                  w=[('wd', i)], grp=('wd', i))

        P.mark('A2')
        load_expert(0)
        with ExitStack() as B0:
            psT = ps(B0, "psT_b", [128, 1024], BF16)
            pR = [ps(B0, "pR%d" % i, [128, 512]) for i in range(2)]
            hn2 = [sb(B0, "hn2_%d" % i, [128, D], BF16) for i in range(2)]
            lg_all = sb(B0, "lg_all", [128, NT, 36], F32)
            em = sb(B0, "em", [128, 32], F32)
            e1 = sb(B0, "e1", [128, 32], F32)
            e2 = sb(B0, "e2", [128, 32], F32)
            ge = sb(B0, "ge", [128, 4], F32)
            oh = sb(B0, "oh", [128, 4], F32)
            top8 = sb(B0, "top8", [128, 8], F32)
            rt = sb(B0, "rt", [128, 16], F32)
            def b0_a(t):
                yk = ('yacc', t)
                P.add('sp', lambda e: e.dma_start(out=yacc[:, t, :], in_=ns.x1_d[t * 128:(t + 1) * 128, :]),
                      r=[('x1d', t)], w=[yk], grp=('yl', t))
                rstd, k = rms_rstd(yacc[:, t, :], 128, D, [yk])
                hb, hbk = hn2[t % 2], ('hn2', t % 2)
                P.add('dve', lambda e: e.scalar_tensor_tensor(out=hb[:], in0=yacc[:, t, :], scalar=rstd,
                                                               in1=gffn_b[:], op0=ALU.mult, op1=ALU.mult),
                      r=[yk, k, 'gffn_b'], w=[hbk])

            def b0_b(t):
                hb, hbk = hn2[t % 2], ('hn2', t % 2)
                for c in range(DC):
                    P.add('pe', lambda e, c=c: e.transpose(out=psT[:, c * 128:(c + 1) * 128], in_=hb[:, c * 128:(c + 1) * 128],
                                                           identity=ident[:]), r=[hbk, 'ident'], w=['psTb'])
                P.add('dve', lambda e: e.tensor_copy(out=h2T[:, :, t * 128:(t + 1) * 128],
                                                     in_=psT[:, :].rearrange("p (c t) -> p c t", c=DC)), r=['psTb'], w=[('h2T', t)])
                pr, prk = pR[t % 2], 'pR%d' % (t % 2)
                for c in range(DC):
                    mm(pr[:, 0:36], h2T[:, c, t * 128:(t + 1) * 128], w_rt_t[:, c, :], c == 0, c == DC - 1, [('h2T', t), 'w_rt_t'], [prk])
                P.add('dve', lambda e: e.tensor_tensor(out=lg_all[:, t, :], in0=pr[:, 0:36], in1=brt_b[:], op=ALU.add),
                      r=[prk, 'brt_b'], w=[('lg', t)])

            b0_a(0)
            for t in range(NT):
                if t + 1 < NT:
                    b0_a(t + 1)
                b0_b(t)
            LG = [('lg', t) for t in range(NT)]
            X = mybir.AxisListType.X
            gl = lg_all[:, :, 0:4]
            el4 = lg_all[:, :, 4:36].rearrange("p t (g j) -> p t g j", g=4)
            r16 = {nm: sb(B0, "r_" + nm, [128, NT], F32) for nm in ("gm", "gsum", "pg", "m1", "m2", "d", "r", "den", "w1", "w2")}
            gsh = sb(B0, "gsh", [128, NT, 4], F32)
            gex = sb(B0, "gex", [128, NT, 4], F32)
            pen = sb(B0, "pen", [128, NT, 4], F32)
            emA = sb(B0, "emA", [128, NT, 32], F32)
            emB = sb(B0, "emB", [128, NT, 32], F32)
            eq1 = sb(B0, "eq1", [128, NT, 32], F32)
            eq2 = sb(B0, "eq2", [128, NT, 32], F32)

            def bc(ap2, n):
                return ap2.unsqueeze(2).to_broadcast([128, NT, n])

            P.add('dve', lambda e: e.tensor_reduce(out=r16["gm"][:], in_=gl, axis=X, op=ALU.max), r=LG, w=['gm'])
            P.add('dve', lambda e: e.tensor_tensor(out=gsh[:], in0=gl, in1=bc(r16["gm"][:, :], 4), op=ALU.subtract), r=LG + ['gm'], w=['gsh'])
            P.add('act', lambda e: e.activation(out=gex[:], in_=gsh[:], func=AF.Exp), r=['gsh'], w=['gex'])
            P.add('dve', lambda e: e.tensor_reduce(out=r16["gsum"][:], in_=gex[:], axis=X, op=ALU.add), r=['gex'], w=['gsum'])
            P.add('dve', lambda e: e.reciprocal(out=r16["pg"][:], in_=r16["gsum"][:]), r=['gsum'], w=['pg'])
            P.add('dve', lambda e: e.tensor_scalar(out=pen[:], in0=gsh[:], scalar1=0.0, scalar2=None, op0=ALU.is_ge), r=['gsh'], w=['pen'])
            P.add('dve', lambda e: e.tensor_scalar(out=pen[:], in0=pen[:], scalar1=-1.0, scalar2=BIG, op0=ALU.add, op1=ALU.mult), r=['pen'], w=['pen'])
            P.add('dve', lambda e: e.tensor_tensor(out=emA[:].rearrange("p t (g j) -> p t g j", g=4), in0=el4,
                                                    in1=pen[:, :, :].unsqueeze(3).to_broadcast([128, NT, 4, 8]), op=ALU.add), r=LG + ['pen'], w=['emA'])
            P.add('dve', lambda e: e.tensor_reduce(out=r16["m1"][:], in_=emA[:], axis=X, op=ALU.max), r=['emA'], w=['m1'])
            P.add('dve', lambda e: e.tensor_tensor(out=eq1[:], in0=emA[:], in1=bc(r16["m1"][:, :], 32), op=ALU.is_equal), r=['emA', 'm1'], w=['eq1'])
            P.add('dve', lambda e: e.scalar_tensor_tensor(out=emB[:], in0=eq1[:], scalar=-BIG, in1=emA[:], op0=ALU.mult, op1=ALU.add),
                  r=['eq1', 'emA'], w=['emB'])
            P.add('dve', lambda e: e.tensor_reduce(out=r16["m2"][:], in_=emB[:], axis=X, op=ALU.max), r=['emB'], w=['m2'])
            P.add('dve', lambda e: e.tensor_tensor(out=eq2[:], in0=emB[:], in1=bc(r16["m2"][:, :], 32), op=ALU.is_equal), r=['emB', 'm2'], w=['eq2'])
            P.add('dve', lambda e: e.tensor_tensor(out=r16["d"][:], in0=r16["m2"][:], in1=r16["m1"][:], op=ALU.subtract), r=['m1', 'm2'], w=['d'])
            P.add('act', lambda e: e.activation(out=r16["r"][:], in_=r16["d"][:], func=AF.Exp), r=['d'], w=['r'])
            P.add('dve', lambda e: e.tensor_scalar(out=r16["den"][:], in0=r16["r"][:], scalar1=1.0, scalar2=None, op0=ALU.add), r=['r'], w=['den'])
            P.add('dve', lambda e: e.reciprocal(out=r16["den"][:], in_=r16["den"][:]), r=['den'], w=['den'])
            P.add('dve', lambda e: e.tensor_tensor(out=r16["w1"][:], in0=r16["den"][:], in1=r16["pg"][:], op=ALU.mult), r=['den', 'pg'], w=['w1'])
            P.add('dve', lambda e: e.tensor_tensor(out=r16["w2"][:], in0=r16["w1"][:], in1=r16["r"][:], op=ALU.mult), r=['w1', 'r'], w=['w2'])
            P.add('dve', lambda e: e.tensor_tensor(out=eq1[:], in0=eq1[:], in1=bc(r16["w1"][:, :], 32), op=ALU.mult), r=['eq1', 'w1'], w=['eq1'])
            P.add('dve', lambda e: e.tensor_tensor(out=eq2[:], in0=eq2[:], in1=bc(r16["w2"][:, :], 32), op=ALU.mult), r=['eq2', 'w2'], w=['eq2'])
            P.add('dve', lambda e: e.tensor_tensor(out=cw[:], in0=eq1[:], in1=eq2[:], op=ALU.add), r=['eq1', 'eq2'], w=[('cw', t) for t in range(NT)])
            P.barrier(pe=True)
        P.mark('B0')
        if ns.dbg:
            for nm, t_, shp, dt in (("d_h2T", h2T, [128, DC, TO], BF16), ("d_cw", cw, [128, NT, N_EXP], F32), ("d_x1", yacc, [128, NT, D], F32)):
                dd = nc.dram_tensor(nm, shp, dt, kind="ExternalOutput").ap()
                P.add('sp', lambda e, dd=dd, t_=t_: e.dma_start(out=dd, in_=t_[:]),
                      r=[('h2T', t) for t in range(NT)] + [('cw', t) for t in range(NT)] + [('yacc', t) for t in range(NT)], w=['outdone'], grp='out')
            P.barrier(pe=True)
        with ExitStack() as B1:
            pG = [ps(B1, "pG%d" % i, [128, 512]) for i in range(2)]
            pU = [ps(B1, "pU%d" % i, [128, 512]) for i in range(2)]
            pY = [ps(B1, "pY%d" % i, [128, 1024]) for i in range(2)]
            steps = [(e_, G) for e_ in range(N_EXP) for G in range(NG)]
            yi = [0]

            def bufs(si):
                return s_act[si % 2], ('s_act', si % 2), a_act[si % 2], ('a_act', si % 2)

            def gu(si, fc):
                e_, G = steps[si]
                i = e_ % 2
                if G == 0 and fc == 1 and e_ + 1 < N_EXP:
                    load_expert(e_ + 1)
                ts = slice(G * 512, (G + 1) * 512)
                hk = [('h2T', G * 4 + t) for t in range(4)]
                sa, sak, aa, aak = bufs(si)
                for c in range(DC):
                    mm(pG[fc][:, :], wg[i][:, c, fc * 128:(fc + 1) * 128], h2T[:, c, ts], c == 0, c == DC - 1, [('wg', i)] + hk, ['pG%d' % fc])
                P.add('act', lambda e: e.activation(out=sa[:, fc, :], in_=pG[fc][:, :], func=AF.Silu), r=['pG%d' % fc], w=[sak + (fc,)])
                for c in range(DC):
                    mm(pU[fc][:, :], wu[i][:, c, fc * 128:(fc + 1) * 128], h2T[:, c, ts], c == 0, c == DC - 1, [('wu', i)] + hk, ['pU%d' % fc])
                P.add('dve', lambda e: e.tensor_tensor(out=aa[:, fc, :], in0=pU[fc][:, :], in1=sa[:, fc, :], op=ALU.mult),
                      r=['pU%d' % fc, sak + (fc,)], w=[aak + (fc,)])

            def down(si):
                e_, G = steps[si]
                i = e_ % 2
                sa, sak, aa, aak = bufs(si)
                for t in range(4):
                    tile = G * 4 + t
                    py, pyk = pY[yi[0] % 2], 'pY%d' % (yi[0] % 2)
                    yt, ytk = ytmp[yi[0] % 2], ('ytmp', yi[0] % 2)
                    yi[0] += 1
                    for hf in range(2):
                        for fc in range(2):
                            mm(py[:, hf * 512:(hf + 1) * 512], aa[:, fc, t * 128:(t + 1) * 128], wd[i][:, fc, hf * 512:(hf + 1) * 512],
                               fc == 0, fc == 1, [aak + (fc,), ('wd', i)], [pyk])
                    P.add('act', lambda e, tile=tile, py=py, yt=yt: e.activation(
                        out=yt[:], in_=py[:, :], func=AF.Identity, scale=cw[:, tile, e_:e_ + 1]),
                        r=[pyk, ('cw', tile)], w=[ytk])
                    P.add('pool' if t % 2 == 0 else 'dve', lambda e, tile=tile, yt=yt: e.tensor_tensor(
                        out=yacc[:, tile, :], in0=yacc[:, tile, :], in1=yt[:], op=ALU.add),
                        r=[ytk, ('yacc', tile)], w=[('yacc', tile)])

            ns_ = len(steps)
            gu(0, 0)
            gu(0, 1)
            for si in range(ns_):
                if si + 1 < ns_:
                    gu(si + 1, 0)
                down(si)
                if si + 1 < ns_:
                    gu(si + 1, 1)
            P.mark('B1')
            for t in range(NT):
                yk = ('yacc', t)
                rstd, k = rms_rstd(yacc[:, t, :], 128, D, [yk])
                P.add('dve', lambda e, t=t, rstd=rstd: e.scalar_tensor_tensor(out=yacc[:, t, :], in0=yacc[:, t, :], scalar=rstd,
                                                                             in1=gfin_b[:], op0=ALU.mult, op1=ALU.mult),
                      r=[yk, k, 'gfin_b'], w=[yk])
                P.add('sp', lambda e, t=t: e.dma_start(out=ns.out[t * 128:(t + 1) * 128, :], in_=yacc[:, t, :]), r=[yk], w=['outdone'], grp='out')


def make_in_maps(cfg, inputs):
    f32 = np.float32
    x = np.asarray(inputs["x"], f32)
    B, S, _ = x.shape
    mem = np.asarray(inputs["mem"], f32)
    pos = np.asarray(inputs["positions"], np.int32)
    L = 0
    w_in = np.asarray(inputs["w_in"], f32)[L]
    w_uq = np.asarray(inputs["w_uq"], f32)[L]
    idx_n = np.concatenate([np.arange(h * 192, h * 192 + 128) for h in range(NH)])
    idx_r = np.concatenate([np.arange(h * 192 + 128, h * 192 + 192) for h in range(NH)])
    idx_rs = np.concatenate([np.concatenate([np.arange(h * 192 + 160, h * 192 + 192), np.arange(h * 192 + 128, h * 192 + 160)]) for h in range(NH)])
    krs = np.concatenate([np.arange(1152 + 32, 1152 + 64), np.arange(1152, 1152 + 32)])
    c = np.ascontiguousarray
    invf = (1.0 / (10000.0 ** (np.arange(0, 64, 2, dtype=np.float32) / 64.0))).astype(f32)
    invf2 = np.concatenate([invf, invf]).astype(f32)
    shared = {
        "invf_col": c(invf2.reshape(64, 1)), "invf_row": c(invf2.reshape(1, 64)),
        "ident": np.eye(128, dtype=ml_dtypes.bfloat16),
        "g_mix": c(np.asarray(inputs["mix_norm_g"], f32)[L].reshape(1, D)),
        "g_mem": c(np.asarray(inputs["mem_norm_g"], f32)[L].reshape(1, D)),
        "g_ffn": c(np.asarray(inputs["ffn_norm_g"], f32)[L].reshape(1, D)),
        "g_fin": c(np.asarray(inputs["final_norm_g"], f32).reshape(1, D)),
        "g_kv": c(np.asarray(inputs["kv_norm_g"], f32)[L].reshape(1, KVR)),
        "g_q": c(np.asarray(inputs["q_norm_g"], f32)[L].reshape(3, 128).T),
        "p_scale": c(np.asarray(inputs["pool_scale"], f32)[L].reshape(4, 128).T),
        "gate_b": c(np.asarray(inputs["gate_b"], f32)[L].reshape(24, 128).T),
        "b_rt": c(np.concatenate([np.asarray(inputs["b_router_group"], f32)[L], np.asarray(inputs["b_router_expert"], f32)[L]]).reshape(1, 36)),
        "w_in_pool": c(w_in[:, 0:512]), "w_in_qd": c(w_in[:, 512:896]),
        "w_in_kvx": c(np.concatenate([w_in[:, 896:1152], w_in[:, 1152:1216], w_in[:, krs]], axis=1)),
        "w_in_xq": c(w_in[:, 1216:1728]), "w_in_gate": c(w_in[:, 1728:4800]),
        "w_uq_n": c(w_uq[:, idx_n]), "w_uq_r": c(w_uq[:, idx_r]), "w_uq_rs": c(w_uq[:, idx_rs]),
        "w_uk": c(np.asarray(inputs["w_uk"], f32)[L]), "w_uv": c(np.asarray(inputs["w_uv"], f32)[L]),
        "pool_w": c(np.asarray(inputs["pool_w"], f32)[L]), "w_mem_kv": c(np.asarray(inputs["w_mem_kv"], f32)[L]),
        "w_br_pool": c(np.asarray(inputs["w_br_pool"], f32)[L]), "w_br_mla": c(np.asarray(inputs["w_br_mla"], f32)[L]),
        "w_br_mem": c(np.asarray(inputs["w_br_mem"], f32)[L]), "w_out": c(np.asarray(inputs["w_out"], f32)[L]),
        "w_rt": c(np.concatenate([np.asarray(inputs["w_router_group"], f32)[L], np.asarray(inputs["w_router_expert"], f32)[L]], axis=1)),
        "w_gate_e": c(np.asarray(inputs["w_gate_e"], f32)[L]), "w_up_e": c(np.asarray(inputs["w_up_e"], f32)[L]),
        "w_down_e": c(np.asarray(inputs["w_down_e"], f32)[L]),
    }
    tri = np.tril(np.ones((128, 128), f32)).T
    in_maps, metas = [], []
    for core in range(2 * B):
        b, half = core // 2, core % 2
        gs = own_groups(cfg, half)
        rows = np.concatenate([np.arange(g * 512, (g + 1) * 512) for g in gs])
        halo = np.zeros((cfg.NG * 16, D), f32)
        invc = np.zeros((4, cfg.TO), f32)
        mk = np.zeros((cfg.NG * 8, 128, 512), f32)
        for l, g in enumerate(gs):
            if g > 0:
                halo[l * 16:(l + 1) * 16] = x[b, g * 512 - 16:g * 512]
            tg = np.arange(g * 512, (g + 1) * 512)
            for wi, wdw in enumerate((2, 4, 8, 16)):
                invc[wi, l * 512:(l + 1) * 512] = 1.0 / np.minimum(tg + 1, wdw)
            kmax = 8 * (l + 1)
            for j in range(8):
                kb = kmax - 8 + j
                for qs in range(4):
                    qb = g * 4 + qs
                    if kb < qb:
                        mk[l * 8 + j, :, qs * 128:(qs + 1) * 128] = 1.0
                    elif kb == qb:
                        mk[l * 8 + j, :, qs * 128:(qs + 1) * 128] = tri
        m = dict(shared)
        m.update({
            "x_seq": c(x[b]), "x_own": c(x[b][rows]), "x_halo": halo,
            "pos_seq": c(pos[b].reshape(cfg.NB, 128).T), "pos_own": c(pos[b][rows].reshape(1, cfg.TO)),
            "mem": c(mem[b]), "inv_cnt": invc, "masks": mk.astype(ml_dtypes.bfloat16),
        })
        in_maps.append(m)
        metas.append((b, rows))
    return in_maps, metas


_CACHE = {}


def kernel(**inputs):
    x = np.asarray(inputs["x"])
    B, S, _ = x.shape
    cfg = Cfg(S)
    if S not in _CACHE:
        _CACHE[S] = build_program(cfg)
    nc, info = _CACHE[S]
    in_maps, metas = make_in_maps(cfg, inputs)
    res = run_bass_kernel_spmd(nc, in_maps, core_ids=list(range(2 * B)))
    outp = np.zeros((B, S, D), np.float32)
    for core, (b, rows) in enumerate(metas):
        outp[b, rows] = np.asarray(res.results[core]["out"], np.float32)
    return outp
```
